# Optimizing a Trainium2 kernel written in Bass

```python
import jax, jax.numpy as jnp
from jax import lax
import numpy as np

D_MODEL = 1024
BATCH = 4
SEQ = 4096
DEPTH = 4

N_MIXERS = 4
N_MEM = 256
EPS = 1e-6
NEG = -1e30
D_FF = ((8 * D_MODEL // 3 + 255) // 256) * 256

POOL_WINDOWS = (2, 4, 8, 16)
POOL_GROUP = D_MODEL // len(POOL_WINDOWS)

NSA_HEADS = 16
NSA_KV_HEADS = 4
NSA_HEAD_DIM = D_MODEL // NSA_HEADS
NSA_CMP_BLOCK = 32
NSA_CMP_STRIDE = 16
NSA_SEL_BLOCK = 64
NSA_N_SELECT = 16
NSA_WINDOW = 512
NSA_CMP_HIDDEN = 4 * NSA_HEAD_DIM
NSA_Q_BLOCK = 64
NSA_KV_WIDTH = NSA_KV_HEADS * NSA_HEAD_DIM
NSA_FORCE = 1e4
NSA_IN_WIDTH = D_MODEL + 6 * NSA_KV_WIDTH + 3 * NSA_HEADS

GLA_HEADS = 4
GLA_DK = D_MODEL // 2 // GLA_HEADS
GLA_DV = D_MODEL // GLA_HEADS
GLA_GATE_RANK = 16
GLA_TAU = 16.0
GLA_CHUNK = 64
GLA_IN_WIDTH = 2 * GLA_HEADS * GLA_DK + 2 * GLA_HEADS * GLA_DV + GLA_GATE_RANK

CONV_WIDTH = 31

XATTN_HEADS = 4
XATTN_HEAD_DIM = D_MODEL // XATTN_HEADS

kernel_name = "hybrid_pool_nsa_gla_conv_macaron"

F32 = jnp.float32


def n_layers_of(m):
    return len(range(m, DEPTH, N_MIXERS))


def rms_norm(x, g):
    xf = x.astype(F32)
    y = xf * lax.rsqrt(jnp.mean(xf * xf, axis=-1, keepdims=True) + EPS)
    return (y * g.astype(F32)).astype(x.dtype)


def swiglu_ffn(h, w_in, w_out):
    g, u = jnp.split(h @ w_in, 2, axis=-1)
    return (jax.nn.silu(g) * u) @ w_out


def pool_mixer(h, w, b, scale):
    B, S, D = h.shape
    hf = h.astype(F32)
    c = jnp.pad(jnp.cumsum(hf, axis=1), ((0, 0), (1, 0), (0, 0)))
    t = jnp.arange(S)
    groups = []
    for gi, win in enumerate(POOL_WINDOWS):
        cg = c[:, :, gi * POOL_GROUP:(gi + 1) * POOL_GROUP]
        start = jnp.maximum(t + 1 - win, 0)
        win_sum = cg[:, 1:] - jnp.take(cg, start, axis=1)
        cnt = (t + 1 - start).astype(F32)[None, :, None]
        groups.append(win_sum / cnt - hf[:, :, gi * POOL_GROUP:(gi + 1) * POOL_GROUP])
    p = jnp.stack(groups, axis=2)
    y = jnp.einsum('bsgc,gcd->bsgd', p, w.astype(F32)).reshape(B, S, D) + b.astype(F32)
    return (y * scale.astype(F32)).astype(h.dtype)


def nsa_mixer(h, w_in, cmp_pos, cmp_w1, cmp_w2, w_o):
    B, S, D = h.shape
    H, G, dh = NSA_HEADS, NSA_KV_HEADS, NSA_HEAD_DIM
    R = H // G
    SEL = NSA_SEL_BLOCK
    proj = (h @ w_in).astype(F32)
    splits = np.cumsum([D] + [NSA_KV_WIDTH] * 6).tolist()
    q, kc, vc, ks, vs, kw, vw, gates = jnp.split(proj, splits, axis=-1)
    q = q.reshape(B, S, G, R, dh).transpose(0, 2, 3, 1, 4) * dh ** -0.5
    gates = jax.nn.sigmoid(gates).reshape(B, S, G, R, 3).transpose(0, 2, 3, 1, 4)

    def kv_heads(a):
        return a.reshape(B, S, G, dh).transpose(0, 2, 1, 3)

    n_cmp = (S - NSA_CMP_BLOCK) // NSA_CMP_STRIDE + 1
    cmp_idx = np.arange(n_cmp)[:, None] * NSA_CMP_STRIDE + np.arange(NSA_CMP_BLOCK)[None, :]

    def compress(a, pos, w1, w2):
        blk = (a[:, :, cmp_idx] + pos.astype(F32)).reshape(B, G, n_cmp, NSA_CMP_BLOCK * dh)
        return jax.nn.gelu(blk @ w1.astype(F32)) @ w2.astype(F32)

    k_cmp = compress(kv_heads(kc), cmp_pos[0], cmp_w1[0], cmp_w2[0])
    v_cmp = compress(kv_heads(vc), cmp_pos[1], cmp_w1[1], cmp_w2[1])
    cmp_end = jnp.asarray(cmp_idx[:, -1])

    n_slc = S // SEL
    cs = np.arange(n_cmp) * NSA_CMP_STRIDE
    ss = np.arange(n_slc) * SEL
    ov = np.clip(np.minimum(cs[:, None] + NSA_CMP_BLOCK, ss[None, :] + SEL)
                 - np.maximum(cs[:, None], ss[None, :]), 0, None) / NSA_CMP_BLOCK
    ov = jnp.asarray(ov, F32)
    k_eff = min(NSA_N_SELECT, n_slc)
    blk_ids = jnp.arange(n_slc)

    ks_blk = kv_heads(ks).reshape(B, G, n_slc, SEL, dh)
    vs_blk = kv_heads(vs).reshape(B, G, n_slc, SEL, dh)
    kw_pad = jnp.pad(kv_heads(kw), ((0, 0), (0, 0), (NSA_WINDOW, 0), (0, 0)))
    vw_pad = jnp.pad(kv_heads(vw), ((0, 0), (0, 0), (NSA_WINDOW, 0), (0, 0)))
    bi = jnp.arange(B)[:, None, None, None]
    gi = jnp.arange(G)[None, :, None, None]
    Qb = min(NSA_Q_BLOCK, S)
    n_qb = S // Qb

    def block(qi):
        s0 = qi * Qb
        t = s0 + jnp.arange(Qb)
        qb = lax.dynamic_slice_in_dim(q, s0, Qb, axis=3)
        gb = lax.dynamic_slice_in_dim(gates, s0, Qb, axis=3)
        vis = cmp_end[None, :] <= t[:, None]
        sc = jnp.where(vis, jnp.einsum('bgrqd,bgnd->bgrqn', qb, k_cmp), NEG)
        p_cmp = jax.nn.softmax(sc, axis=-1) * jnp.any(vis, axis=-1)[:, None]
        o_cmp = jnp.einsum('bgrqn,bgnd->bgrqd', p_cmp, v_cmp)
        imp = jnp.einsum('bgrqn,nm->bgqm', p_cmp, ov)
        cur = t // SEL
        forced = (blk_ids[None, :] == 0) | (blk_ids[None, :] == cur[:, None]) | (blk_ids[None, :] == cur[:, None] - 1)
        imp = jnp.where(forced, NSA_FORCE, imp)
        imp = jnp.where(blk_ids[None, :] * SEL <= t[:, None], imp, -1.0)
        _, idx = lax.top_k(imp, k_eff)
        ksel = ks_blk[bi, gi, idx].reshape(B, G, Qb, k_eff * SEL, dh)
        vsel = vs_blk[bi, gi, idx].reshape(B, G, Qb, k_eff * SEL, dh)
        pos = (idx[..., None] * SEL + jnp.arange(SEL)).reshape(B, G, Qb, k_eff * SEL)
        sc = jnp.einsum('bgrqd,bgqkd->bgrqk', qb, ksel)
        sc = jnp.where((pos <= t[:, None])[:, :, None], sc, NEG)
        o_slc = jnp.einsum('bgrqk,bgqkd->bgrqd', jax.nn.softmax(sc, axis=-1), vsel)
        kwin = lax.dynamic_slice_in_dim(kw_pad, s0, NSA_WINDOW + Qb, axis=2)
        vwin = lax.dynamic_slice_in_dim(vw_pad, s0, NSA_WINDOW + Qb, axis=2)
        wpos = s0 - NSA_WINDOW + jnp.arange(NSA_WINDOW + Qb)
        wmask = (wpos[None, :] <= t[:, None]) & (wpos[None, :] > t[:, None] - NSA_WINDOW) & (wpos[None, :] >= 0)
        sc = jnp.where(wmask, jnp.einsum('bgrqd,bgkd->bgrqk', qb, kwin), NEG)
        o_win = jnp.einsum('bgrqk,bgkd->bgrqd', jax.nn.softmax(sc, axis=-1), vwin)
        return gb[..., 0:1] * o_cmp + gb[..., 1:2] * o_slc + gb[..., 2:3] * o_win

    o = lax.map(block, jnp.arange(n_qb))
    o = o.transpose(1, 0, 4, 2, 3, 5).reshape(B, S, H * dh)
    return o.astype(h.dtype) @ w_o


def gla_mixer(h, w_in, w_gate_up, b_gate, norm_g, w_o):
    B, S, D = h.shape
    Hh, dk, dv, C = GLA_HEADS, GLA_DK, GLA_DV, GLA_CHUNK
    proj = (h @ w_in).astype(F32)
    qw, vw = Hh * dk, Hh * dv
    q, k, v, og, gdown = jnp.split(proj, [qw, 2 * qw, 2 * qw + vw, 2 * qw + 2 * vw], axis=-1)
    log_a = jax.nn.log_sigmoid(gdown @ w_gate_up.astype(F32) + b_gate.astype(F32)) / GLA_TAU
    nc = S // C

    def heads(a, d):
        return a.reshape(B, nc, C, Hh, d).transpose(0, 3, 1, 2, 4)

    q = heads(q, dk) * dk ** -0.5
    k = heads(k, dk)
    v = heads(v, dv)
    b = jnp.cumsum(heads(log_a, dk), axis=3)
    b_last = b[:, :, :, -1:, :]
    q_t = q * jnp.exp(b)
    k_t = k * jnp.exp(-b)
    causal = jnp.tril(jnp.ones((C, C), dtype=bool))
    A = jnp.where(causal, jnp.einsum('bhncd,bhnsd->bhncs', q_t, k_t), 0.0)
    o_intra = jnp.einsum('bhncs,bhnsv->bhncv', A, v)
    kv = jnp.einsum('bhncd,bhncv->bhndv', k * jnp.exp(b_last - b), v)
    decay = jnp.exp(b_last[:, :, :, 0, :])

    def step(state, inp):
        kv_n, dec_n = inp
        return dec_n[..., None] * state + kv_n, state

    init = jnp.zeros((B, Hh, dk, dv), F32)
    _, s_prev = lax.scan(step, init, (kv.transpose(2, 0, 1, 3, 4), decay.transpose(2, 0, 1, 3)))
    s_prev = s_prev.transpose(1, 2, 0, 3, 4)
    o = o_intra + jnp.einsum('bhncd,bhndv->bhncv', q_t, s_prev)
    o = rms_norm(o.transpose(0, 2, 3, 1, 4).reshape(B, S, Hh, dv), norm_g)
    o = o.reshape(B, S, Hh * dv) * jax.nn.silu(og)
    return o.astype(h.dtype) @ w_o


def conv_mixer(h, w_in, b_in, w_dw, b_dw, ln_g, ln_b, w_out, b_out):
    D = h.shape[-1]
    a, gate = jnp.split(h @ w_in + b_in, 2, axis=-1)
    u = a * jax.nn.sigmoid(gate)
    u = jnp.pad(u, ((0, 0), (CONV_WIDTH - 1, 0), (0, 0)))
    u = lax.conv_general_dilated(u, w_dw[:, None, :].astype(u.dtype), window_strides=(1,), padding='VALID',
                                 dimension_numbers=('NWC', 'WIO', 'NWC'), feature_group_count=D) + b_dw
    uf = u.astype(F32)
    mu = jnp.mean(uf, axis=-1, keepdims=True)
    var = jnp.mean(jnp.square(uf - mu), axis=-1, keepdims=True)
    uf = (uf - mu) * lax.rsqrt(var + EPS) * ln_g.astype(F32) + ln_b.astype(F32)
    return jax.nn.silu(uf).astype(h.dtype) @ w_out + b_out


def memory_cross_attention(h, mem_n, w_q, w_kv, w_o):
    B, S, D = h.shape
    N = mem_n.shape[1]
    Hx, dh = XATTN_HEADS, XATTN_HEAD_DIM
    q = (h @ w_q).astype(F32).reshape(B, S, Hx, dh) * dh ** -0.5
    k, v = jnp.split((mem_n @ w_kv).astype(F32), 2, axis=-1)
    k = k.reshape(B, N, Hx, dh)
    v = v.reshape(B, N, Hx, dh)
    p = jax.nn.softmax(jnp.einsum('bqhd,bkhd->bhqk', q, k), axis=-1)
    o = jnp.einsum('bhqk,bkhd->bqhd', p, v).reshape(B, S, D)
    return o.astype(h.dtype) @ w_o


def setup_inputs(seed: int = 0) -> dict:
    key = jax.random.key(seed)
    keys = iter(jax.random.split(key, 64))
    D, F, L = D_MODEL, D_FF, DEPTH
    nA, nB, nC, nD = (n_layers_of(m) for m in range(N_MIXERS))

    def w(shape, fan_in):
        return jax.random.normal(next(keys), shape, F32) * fan_in ** -0.5

    def gain(shape):
        return 1.0 + 0.02 * jax.random.normal(next(keys), shape, F32)

    def small(shape, s=0.02):
        return s * jax.random.normal(next(keys), shape, F32)

    return {
        "x": jax.random.normal(next(keys), (BATCH, SEQ, D), F32),
        "mem": jax.random.normal(next(keys), (BATCH, N_MEM, D), F32),
        "ffn1_norm": gain((L, D)),
        "ffn1_w_in": w((L, D, 2 * F), D),
        "ffn1_w_out": w((L, F, D), F),
        "mix_norm": gain((L, D)),
        "xattn_norm": gain((L, D)),
        "mem_norm": gain((L, D)),
        "xattn_w_q": w((L, D, D), D),
        "xattn_w_kv": w((L, D, 2 * D), D),
        "xattn_w_o": w((L, D, D), D),
        "ffn2_norm": gain((L, D)),
        "ffn2_w_in": w((L, D, 2 * F), D),
        "ffn2_w_out": w((L, F, D), F),
        "pool_w": w((nA, len(POOL_WINDOWS), POOL_GROUP, POOL_GROUP), POOL_GROUP),
        "pool_b": small((nA, D)),
        "pool_scale": 1.0 + 0.1 * jax.random.normal(next(keys), (nA, D), F32),
        "nsa_w_in": w((nB, D, NSA_IN_WIDTH), D),
        "nsa_cmp_pos": small((nB, 2, NSA_CMP_BLOCK, NSA_HEAD_DIM), 0.1),
        "nsa_cmp_w1": w((nB, 2, NSA_CMP_BLOCK * NSA_HEAD_DIM, NSA_CMP_HIDDEN), NSA_CMP_BLOCK * NSA_HEAD_DIM),
        "nsa_cmp_w2": w((nB, 2, NSA_CMP_HIDDEN, NSA_HEAD_DIM), NSA_CMP_HIDDEN),
        "nsa_w_o": w((nB, D, D), D),
        "gla_w_in": w((nC, D, GLA_IN_WIDTH), D),
        "gla_w_gate_up": w((nC, GLA_GATE_RANK, GLA_HEADS * GLA_DK), GLA_GATE_RANK),
        "gla_b_gate": small((nC, GLA_HEADS * GLA_DK)),
        "gla_norm": gain((nC, GLA_DV)),
        "gla_w_o": w((nC, D, D), D),
        "conv_w_in": w((nD, D, 2 * D), D),
        "conv_b_in": small((nD, 2 * D)),
        "conv_dw": w((nD, CONV_WIDTH, D), CONV_WIDTH),
        "conv_b_dw": small((nD, D)),
        "conv_ln_g": gain((nD, D)),
        "conv_ln_b": small((nD, D)),
        "conv_w_out": w((nD, D, D), D),
        "conv_b_out": small((nD, D)),
        "final_norm": gain((D,)),
    }


def reference(x, mem, ffn1_norm, ffn1_w_in, ffn1_w_out, mix_norm, xattn_norm, mem_norm,
              xattn_w_q, xattn_w_kv, xattn_w_o, ffn2_norm, ffn2_w_in, ffn2_w_out,
              pool_w, pool_b, pool_scale,
              nsa_w_in, nsa_cmp_pos, nsa_cmp_w1, nsa_cmp_w2, nsa_w_o,
              gla_w_in, gla_w_gate_up, gla_b_gate, gla_norm, gla_w_o,
              conv_w_in, conv_b_in, conv_dw, conv_b_dw, conv_ln_g, conv_ln_b, conv_w_out, conv_b_out,
              final_norm):
    for i in range(DEPTH):
        m, j = i % N_MIXERS, i // N_MIXERS
        x = x + 0.5 * swiglu_ffn(rms_norm(x, ffn1_norm[i]), ffn1_w_in[i], ffn1_w_out[i])
        h = rms_norm(x, mix_norm[i])
        if m == 0:
            y = pool_mixer(h, pool_w[j], pool_b[j], pool_scale[j])
        elif m == 1:
            y = nsa_mixer(h, nsa_w_in[j], nsa_cmp_pos[j], nsa_cmp_w1[j], nsa_cmp_w2[j], nsa_w_o[j])
        elif m == 2:
            y = gla_mixer(h, gla_w_in[j], gla_w_gate_up[j], gla_b_gate[j], gla_norm[j], gla_w_o[j])
        else:
            y = conv_mixer(h, conv_w_in[j], conv_b_in[j], conv_dw[j], conv_b_dw[j],
                           conv_ln_g[j], conv_ln_b[j], conv_w_out[j], conv_b_out[j])
        x = x + y
        x = x + memory_cross_attention(rms_norm(x, xattn_norm[i]), rms_norm(mem, mem_norm[i]),
                                       xattn_w_q[i], xattn_w_kv[i], xattn_w_o[i])
        x = x + 0.5 * swiglu_ffn(rms_norm(x, ffn2_norm[i]), ffn2_w_in[i], ffn2_w_out[i])
    return rms_norm(x, final_norm)
```

```python
from concourse.bass_utils import run_bass_kernel_spmd

import numpy as np
import concourse.bass as bass
import concourse.mybir as mybir

F32 = mybir.dt.float32
BF16 = mybir.dt.bfloat16
AF = mybir.ActivationFunctionType
ALU = mybir.AluOpType
AX = mybir.AxisListType

ENGS = ("pe", "act", "dve", "pool", "sp")


class _St:
    __slots__ = ("w", "r")

    def __init__(self, w=None, r=None):
        self.w = w
        self.r = dict(r or {})

    def copy(self):
        return _St(self.w, self.r)


class DSem:
    def __init__(self, h):
        self.h, self.cnt = h, 0


class Tile:
    def __init__(self, ctx, name, t, space):
        self.ctx, self.name, self.t, self.space = ctx, name, t, space
        self.whole = _St()
        self.regions = {}
        self.dsem = None
        self.dcnt = 0

    def __getitem__(self, idx):
        return self.t[idx]

    def st(self, key):
        if key is None:
            return None
        if key not in self.regions:
            self.regions[key] = self.whole.copy()
        return self.regions[key]


class Ctx:
    def __init__(self, nc):
        self.nc = nc
        self.E = {"pe": nc.tensor, "act": nc.scalar, "dve": nc.vector, "pool": nc.gpsimd, "sp": nc.sync}
        self.ops = []
        self.ecount = {e: 0 for e in ENGS}
        self.tiles = []
        self.same_engine_sync = {"pe": False, "act": True, "dve": True, "pool": True, "sp": False}
        self.bar = {e: set() for e in ENGS}
        self.last = {}

    def barrier(self):
        deps = {("e", e, i) for e, i in self.last.items()}
        for d in getattr(self, "dall", []):
            if d.cnt:
                deps.add(("d", d, d.cnt))
        for e in ENGS:
            self.bar[e] |= deps

    class _Phase:
        def __init__(self, c):
            self.c = c

        def __enter__(self):
            nc = self.c.nc
            self.sv = (nc.sbuf_base, nc.sbuf_top, nc.psum_base, nc.psum_top)
            self.ntiles = len(self.c.tiles)
            return self

        def __exit__(self, *a):
            nc = self.c.nc
            self.c.barrier()
            for T in self.c.tiles[self.ntiles:]:
                if T.space != "dram" and T.dsem is not None:
                    self.c.dfree.append(T.dsem)
                    T.dsem = None
            del self.c.tiles[self.ntiles:]
            nc.sbuf_base, nc.sbuf_top, nc.psum_base, nc.psum_top = self.sv

    def phase(self):
        return Ctx._Phase(self)

    def _nm(self, name):
        self._uid = getattr(self, "_uid", 0) + 1
        return f"{name}_{self._uid}"

    def get_dsem(self):
        if not hasattr(self, "dfree"):
            self.dfree, self.dall = [], []
        if self.dfree:
            return self.dfree.pop()
        d = DSem(self.nc.alloc_semaphore(self._nm("dsem")))
        self.dall.append(d)
        return d

    def sbuf(self, name, shape, dtype):
        name = self._nm(name)
        t = self.nc.alloc_sbuf_tensor(name, list(shape), dtype)
        T = Tile(self, name, t, "sbuf")
        self.tiles.append(T)
        return T

    def psum(self, name, shape, dtype=F32):
        name = self._nm(name)
        t = self.nc.alloc_psum_tensor(name, list(shape), dtype)
        T = Tile(self, name, t, "psum")
        self.tiles.append(T)
        return T

    def dram(self, name, shape, dtype, kind="Internal"):
        t = self.nc.dram_tensor(name, list(shape), dtype, kind=kind)
        T = Tile(self, name, t.ap(), "dram")
        self.tiles.append(T)
        return T

    def _collect(self, eng, reads, writes):
        deps = set()

        def states(T, key):
            if key is None:
                return [T.whole] + list(T.regions.values())
            return [T.st(key)]

        for (T, key) in reads:
            for s in states(T, key):
                if s.w is not None:
                    deps.add(s.w)
        for (T, key) in writes:
            for s in states(T, key):
                if s.w is not None:
                    deps.add(s.w)
                for d in s.r.values():
                    deps.add(d)
        return deps

    def _update(self, me_r, me_w, rkey, reads, writes):
        for (T, key) in reads:
            if key is None:
                T.whole.r[rkey] = me_r
                for s in T.regions.values():
                    s.r[rkey] = me_r
            else:
                T.st(key).r[rkey] = me_r
        for (T, key) in writes:
            if key is None:
                T.whole = _St(me_w)
                T.regions = {}
            else:
                s = T.st(key)
                s.w = me_w
                s.r = {}

    def op(self, eng, fn, reads=(), writes=()):
        reads = [(r, None) if isinstance(r, Tile) else r for r in reads]
        writes = [(w, None) if isinstance(w, Tile) else w for w in writes]
        writes = writes + [(T, None) for (T, k) in reads if T.space == "psum"]
        reads = [(T, k) for (T, k) in reads if T.space != "psum"]
        writes = [(T, None) if T.space == "psum" else (T, k) for (T, k) in writes]
        deps = self._collect(eng, reads, writes)
        deps = {(d[0], d[1], d[1].cnt) if d[0] == "d" else d for d in deps}
        idx = self.ecount[eng]
        self.ecount[eng] += 1
        me = ("e", eng, idx)
        if self.bar[eng]:
            deps |= self.bar[eng]
            self.bar[eng] = set()
        self.last[eng] = idx
        if not self.same_engine_sync[eng]:
            deps = {d for d in deps if not (d[0] == "e" and d[1] == eng)}
        else:
            deps = {d for d in deps if not (d == me)}
        self.ops.append(dict(eng=eng, fn=fn, deps=deps, idx=idx, kind="c"))
        self._update(me, me, eng, reads, writes)

    def call(self, eng, method, reads, writes, *args, **kw):
        f = getattr(self.E[eng], method)
        self.op(eng, lambda: f(*args, **kw), reads=reads, writes=writes)

    def mm(self, out, lhsT, rhs, start, stop, reads, writes):
        f = self.nc.tensor.matmul
        self.op("pe", lambda: f(out, lhsT=lhsT, rhs=rhs, start=start, stop=stop), reads=reads, writes=writes)

    def act(self, out, in_, func, reads, writes, **kw):
        f = self.nc.scalar.activation
        self.op("act", lambda: f(out=out, in_=in_, func=func, **kw), reads=reads, writes=writes)

    def ld(self, q, out, in_, reads, writes, **kw):
        f = self.E[q].dma_start
        self.dma(q, lambda: f(out=out, in_=in_, **kw), reads=reads, writes=writes)

    def dma(self, q, fn, reads=(), writes=()):
        reads = [(r, None) if isinstance(r, Tile) else r for r in reads]
        writes = [(w, None) if isinstance(w, Tile) else w for w in writes]
        deps = self._collect(q, reads, writes)
        deps = {(d[0], d[1], d[1].cnt) if d[0] == "d" else d for d in deps}
        idx = self.ecount[q]
        self.ecount[q] += 1
        cands = [T for (T, _) in writes if T.space != "dram"] + [T for (T, _) in reads if T.space != "dram"] \
            + [T for (T, _) in writes] + [T for (T, _) in reads]
        own = cands[0]
        if own.dsem is None:
            own.dsem = self.get_dsem()
        own.dsem.cnt += 16
        me = ("d", own.dsem, own.dsem.cnt)
        if self.bar[q]:
            deps |= self.bar[q]
            self.bar[q] = set()
        deps = {d for d in deps if not (d[0] == "e" and d[1] == q)}
        self.ops.append(dict(eng=q, fn=fn, deps=deps, idx=idx, kind="d", dsem=own.dsem))
        self._update(me, me, ("d", id(own.dsem)), reads, writes)

    def emit(self, final_waits=()):
        nc = self.nc
        sig = {e: set() for e in ENGS}
        for o in self.ops:
            for d in o["deps"]:
                if d[0] == "e":
                    sig[d[1]].add(d[2])
        rank = {}
        for e in ENGS:
            for i, idx in enumerate(sorted(sig[e])):
                rank[(e, idx)] = i + 1
        sem = {e: nc.alloc_semaphore("c_" + e) for e in ENGS if sig[e]}
        seen = {e: {} for e in ENGS}
        nwaits = 0
        for o in self.ops:
            e = o["eng"]
            eng = self.E[e]
            need = {}
            for d in o["deps"]:
                if d[0] == "e":
                    s, v = sem[d[1]], rank[(d[1], d[2])]
                    k = ("e", d[1])
                else:
                    s, v = d[1].h, d[2]
                    k = ("d", id(d[1]))
                if v > need.get(k, (None, 0))[1]:
                    need[k] = (s, v)
            for k, (s, v) in need.items():
                if seen[e].get(k, 0) >= v:
                    continue
                eng.wait_ge(s, v)
                seen[e][k] = v
                nwaits += 1
            ins = o["fn"]()
            if o["kind"] == "d":
                ins.then_inc(o["dsem"].h, 16)
            elif (e, o["idx"]) in rank:
                ins.then_inc(sem[e], 1)
        for d in getattr(self, "dall", []):
            if d.cnt:
                nc.sync.wait_ge(d.h, d.cnt)
        self.stats = dict(nops=len(self.ops), nwaits=nwaits, counts=dict(self.ecount))


D = 1024
FF = 2816
KC = 8
EPS = 1e-6
PIECES = [(0, 6), (6, 6), (12, 5), (17, 5)]


class M:
    def __init__(self, c, S):
        self.c, self.nc, self.S = c, c.nc, S
        self._stg_i = 0

    def consts(self):
        c, nc = self.c, self.nc
        self.onesb = c.sbuf("onesb", [128, 128], BF16)
        self.eps = c.sbuf("eps_t", [128, 1], F32)
        self.stg = [c.sbuf(f"stg{i}", [128, 1024], F32) for i in range(4)]
        c.call("dve", "memset", [], [self.onesb], self.onesb[:, :], 1.0)
        c.call("dve", "memset", [], [self.eps], self.eps[:, :], EPS)

    def load_w(self, dst, dst_ap_fn, W, r0, nrows_chunks, c0, ncols, key=None):
        c, nc = self.c, self.nc
        for kc in range(nrows_chunks):
            for cs in range(0, ncols, 1024):
                cn = min(1024, ncols - cs)
                st = self.stg[self._stg_i % 4]
                self._stg_i += 1
                c.ld("sp", st[:, 0:cn], W[r0 + kc * 128: r0 + (kc + 1) * 128, c0 + cs: c0 + cs + cn], [W], [st])
                c.call("pool", "tensor_copy", [st], [(dst, (key, kc, cs))], out=dst_ap_fn(kc, cs, cn), in_=st[:, 0:cn])

    def load_vec(self, name, v_ap, n):
        c = self.c
        t = c.sbuf(name, [128, n], F32)
        c.ld("sp", t[:, :], v_ap.rearrange("(k p) -> p k", p=128), [], [t], allow_slow_non_contiguous=True)
        return t

    def norm_cols(self, xt, xn, g, ncols, xcols, ocols, ps, sqb, rs, nk=KC, dim=D, gk=None, key=None):
        c, nc = self.c, self.nc
        for k in range(nk):
            s = sqb[k % 2]
            c.act(s[:, 0:ncols], xt[:, k, xcols], AF.Square, [xt], [s])
            c.mm(ps[:, 0:ncols], self.onesb[:, :], s[:, 0:ncols], k == 0, k == nk - 1, [self.onesb, s], [ps])
        c.act(rs[:, 0:ncols], ps[:, 0:ncols], AF.Sqrt, [ps, self.eps], [rs], bias=self.eps[:, 0:1], scale=1.0 / dim)
        c.call("dve", "reciprocal", [rs], [rs], out=rs[:, 0:ncols], in_=rs[:, 0:ncols])
        for k in range(nk):
            c.call("dve", "scalar_tensor_tensor", [xt, g, rs], [(xn, key)], out=xn[:, k, ocols], in0=xt[:, k, xcols],
                   scalar=g[:, (k if gk is None else gk(k)):(k if gk is None else gk(k)) + 1], in1=rs[:, 0:ncols],
                   op0=ALU.mult, op1=ALU.mult)

    def ffn(self, src, dst, g_ap, win, wout, li):
        c, nc, S = self.c, self.nc, self.S
        TG = 256
        H = min(2048, S)
        NTG = H // TG
        with c.phase():
            x = c.sbuf("f_x", [128, KC, H], F32)
            xn = c.sbuf("f_xn", [128, KC, H], BF16)
            g = self.load_vec("f_g", g_ap, KC)
            sqb = [c.sbuf(f"f_sq{i}", [128, TG], BF16) for i in range(2)]
            rstd = [c.sbuf(f"f_rs{i}", [128, TG], F32) for i in range(2)]
            wi = [c.sbuf(f"f_wi{i}", [128, KC, 2, 768], BF16) for i in range(2)]
            wo = [c.sbuf(f"f_wo{i}", [128, 6, D], BF16) for i in range(2)]
            sil = [c.sbuf(f"f_sil{i}", [128, TG], F32) for i in range(2)]
            act = [c.sbuf(f"f_act{i}", [128, 6, TG], BF16) for i in range(2)]
            ph = [c.psum(f"f_ph{i}", [128, 512]) for i in range(4)]
            py = [c.psum(f"f_py{i}", [128, 2, TG]) for i in range(4)]
            pcount = 0
            hcount = 0
            for half in range(S // H):
                hs = half * H
                for k in range(KC):
                    c.ld("sp", x[:, k, :], src[k * 128:(k + 1) * 128, hs:hs + H], [(src, (k, half))], [(x, k)])

                def load_piece(pi, b):
                    f0, nf = PIECES[pi]
                    for h in range(2):
                        self.load_w(wi[b], lambda kc, cs, cn, h=h, b=b: wi[b][:, kc, h, cs:cs + cn], win, 0, KC,
                                    h * FF + f0 * 128, nf * 128, key=h)
                    self.load_w(wo[b], lambda kc, cs, cn, b=b: wo[b][:, kc, cs:cs + cn], wout, f0 * 128, nf, 0, D)

                load_piece(0, pcount % 2)
                for tg in range(NTG):
                    ts = slice(tg * TG, (tg + 1) * TG)
                    self.norm_cols(x, xn, g, TG, ts, ts, ph[tg % 4], sqb, rstd[tg % 2], key=tg)
                for pi, (f0, nf) in enumerate(PIECES):
                    b = pcount % 2
                    pcount += 1
                    if pi + 1 < len(PIECES):
                        load_piece(pi + 1, pcount % 2)
                    for tg in range(NTG):
                        ts = slice(tg * TG, (tg + 1) * TG)
                        a = act[tg % 2]
                        for f in range(nf):
                            pp = (ph[(2 * hcount) % 4], ph[(2 * hcount + 1) % 4])
                            hcount += 1
                            for h in range(2):
                                for k in range(KC):
                                    c.mm(pp[h][:, 0:TG], wi[b][:, k, h, f * 128:(f + 1) * 128], xn[:, k, ts],
                                         k == 0, k == KC - 1, [wi[b], (xn, tg)], [pp[h]])
                            s = sil[hcount % 2]
                            c.act(s[:, :], pp[0][:, 0:TG], AF.Silu, [pp[0]], [s])
                            c.call("dve", "tensor_tensor", [pp[1], s], [(a, f)], out=a[:, f, :], in0=pp[1][:, 0:TG],
                                   in1=s[:, :], op=ALU.mult)
                        for dc in range(KC):
                            for f in range(nf):
                                c.mm(py[dc // 2][:, dc % 2, :], wo[b][:, f, dc * 128:(dc + 1) * 128], a[:, f, :],
                                     f == 0, f == nf - 1, [wo[b], (a, f)], [py[dc // 2]])
                        for k in range(KC):
                            c.call("dve", "scalar_tensor_tensor", [py[k // 2], (x, k)], [(x, k)], out=x[:, k, ts],
                                   in0=py[k // 2][:, k % 2, :], scalar=0.5, in1=x[:, k, ts], op0=ALU.mult, op1=ALU.add)
                for k in range(KC):
                    c.ld("sp", dst[k * 128:(k + 1) * 128, hs:hs + H], x[:, k, :], [(x, k)], [(dst, (k, half))])

    def xattn(self, xs, memT, gx_ap, gm_ap, wq_d, wkv_d, wo_d):
        c, nc, S = self.c, self.nc, self.S
        T = 512
        with c.phase():
            gx = self.load_vec("a_gx", gx_ap, KC)
            gm = self.load_vec("a_gm", gm_ap, KC)
            mem = c.sbuf("a_mem", [128, KC, 256], F32)
            memn = c.sbuf("a_memn", [128, KC, 256], BF16)
            wkv = c.sbuf("a_wkv", [128, KC, 2048], BF16)
            wq = c.sbuf("a_wq", [128, KC, D], BF16)
            wo = c.sbuf("a_wo", [128, KC, D], BF16)
            kT = c.sbuf("a_kT", [128, KC, 256], BF16)
            V = c.sbuf("a_V", [128, 2, D], BF16)
            sqb = [c.sbuf(f"a_sq{i}", [128, T], BF16) for i in range(2)]
            rs = c.sbuf("a_rs", [128, T], F32)
            xt = [c.sbuf(f"a_x{i}", [128, KC, T], F32) for i in range(2)]
            xn = c.sbuf("a_xn", [128, KC, T], BF16)
            qT = c.sbuf("a_qT", [128, KC, T], BF16)
            pT = [c.sbuf(f"a_pT{i}", [128, 2, T], BF16) for i in range(2)]
            rden = c.sbuf("a_rden", [128, T], F32)
            oT = c.sbuf("a_oT", [128, KC, T], BF16)
            ps = [c.psum(f"a_ps{i}", [128, 512]) for i in range(8)]
            pi = [0]

            def nps():
                pi[0] += 1
                return ps[pi[0] % 8]

            for k in range(KC):
                c.ld("sp", mem[:, k, :], memT[k * 128:(k + 1) * 128, :], [memT], [(mem, k)])
            self.load_w(wkv, lambda kc, cs, cn: wkv[:, kc, cs:cs + cn], wkv_d, 0, KC, 0, 2048)
            self.load_w(wq, lambda kc, cs, cn: wq[:, kc, cs:cs + cn], wq_d, 0, KC, 0, D)
            self.load_w(wo, lambda kc, cs, cn: wo[:, kc, cs:cs + cn], wo_d, 0, KC, 0, D)
            self.norm_cols(mem, memn, gm, 256, slice(0, 256), slice(0, 256), nps(), sqb, rs)
            for oc in range(KC):
                p = nps()
                for k in range(KC):
                    c.mm(p[:, 0:256], wkv[:, k, oc * 128:(oc + 1) * 128], memn[:, k, :], k == 0, k == KC - 1, [wkv, memn], [p])
                c.call("dve", "tensor_copy", [p], [(kT, oc)], out=kT[:, oc, :], in_=p[:, 0:256])
            for kc in range(2):
                for nb in range(2):
                    p = nps()
                    for k in range(KC):
                        c.mm(p[:, :], memn[:, k, kc * 128:(kc + 1) * 128], wkv[:, k, D + nb * 512: D + (nb + 1) * 512],
                             k == 0, k == KC - 1, [wkv, memn], [p])
                    c.call("dve", "tensor_copy", [p], [(V, (kc, nb))], out=V[:, kc, nb * 512:(nb + 1) * 512], in_=p[:, :])
            for tg in range(S // T):
                x = xt[tg % 2]
                cs = slice(tg * T, (tg + 1) * T)
                for k in range(KC):
                    c.ld("sp", x[:, k, :], xs[k * 128:(k + 1) * 128, cs], [(xs, (k, tg))], [(x, k)])
                self.norm_cols(x, xn, gx, T, slice(0, T), slice(0, T), nps(), sqb, rs)
                for oc in range(KC):
                    p = nps()
                    for k in range(KC):
                        c.mm(p[:, :], wq[:, k, oc * 128:(oc + 1) * 128], xn[:, k, :], k == 0, k == KC - 1, [wq, xn], [p])
                    c.act(qT[:, oc, :], p[:, :], AF.Copy, [p], [(qT, oc)], scale=1.0 / 16.0)
                for h in range(4):
                    pt = pT[h % 2]
                    for kc in range(2):
                        p = nps()
                        for dc in range(2):
                            c.mm(p[:, :], kT[:, h * 2 + dc, kc * 128:(kc + 1) * 128], qT[:, h * 2 + dc, :], dc == 0, dc == 1,
                                 [kT, (qT, h * 2 + dc)], [p])
                        c.act(pt[:, kc, :], p[:, :], AF.Exp, [p], [(pt, kc)])
                    pd = nps()
                    for kc in range(2):
                        c.mm(pd[:, :], self.onesb[:, :], pt[:, kc, :], kc == 0, kc == 1, [self.onesb, (pt, kc)], [pd])
                    c.act(rden[:, :], pd[:, :], AF.Ln, [pd], [rden])
                    c.act(rden[:, :], rden[:, :], AF.Exp, [rden], [rden], scale=-1.0)
                    for mc in range(2):
                        p = nps()
                        for kc in range(2):
                            c.mm(p[:, :], V[:, kc, h * 256 + mc * 128: h * 256 + (mc + 1) * 128], pt[:, kc, :], kc == 0, kc == 1,
                                 [V, (pt, kc)], [p])
                        c.call("dve", "tensor_tensor", [p, rden], [(oT, h * 2 + mc)], out=oT[:, h * 2 + mc, :], in0=p[:, :],
                               in1=rden[:, :], op=ALU.mult)
                for dc in range(KC):
                    p = nps()
                    for k in range(KC):
                        c.mm(p[:, :], wo[:, k, dc * 128:(dc + 1) * 128], oT[:, k, :], k == 0, k == KC - 1, [wo, (oT, k)], [p])
                    c.call("dve", "tensor_tensor", [p, (x, dc)], [(x, dc)], out=x[:, dc, :], in0=p[:, :], in1=x[:, dc, :], op=ALU.add)
                    c.ld("sp", xs[dc * 128:(dc + 1) * 128, cs], x[:, dc, :], [(x, dc)], [(xs, (dc, tg))])

    def final_norm(self, xs, out, g_ap):
        c, nc, S = self.c, self.nc, self.S
        T = 512
        with c.phase():
            g = self.load_vec("n_g", g_ap, KC)
            sqb = [c.sbuf(f"n_sq{i}", [128, T], BF16) for i in range(2)]
            rs = c.sbuf("n_rs", [128, T], F32)
            xt = [c.sbuf(f"n_x{i}", [128, KC, T], F32) for i in range(2)]
            xo = [c.sbuf(f"n_o{i}", [128, KC, T], F32) for i in range(2)]
            ps = [c.psum(f"n_ps{i}", [128, 512]) for i in range(2)]
            for tg in range(S // T):
                x, o = xt[tg % 2], xo[tg % 2]
                cs = slice(tg * T, (tg + 1) * T)
                for k in range(KC):
                    c.ld("sp", x[:, k, :], xs[k * 128:(k + 1) * 128, cs], [(xs, (k, tg))], [(x, k)])
                self.norm_cols(x, o, g, T, slice(0, T), slice(0, T), ps[tg % 2], sqb, rs)
                for k in range(KC):
                    c.ld("sp", out[k * 128:(k + 1) * 128, cs], o[:, k, :], [o], [(out, (k, tg))])

    def pool(self, xs, g_ap, pw_d, pb_ap, psc_ap, inv_d):
        c, nc, S = self.c, self.nc, self.S
        T = 512
        HL = 16
        WINS = (2, 4, 8, 16)
        with c.phase():
            g = self.load_vec("p_g", g_ap, KC)
            pb = self.load_vec("p_b", pb_ap, KC)
            psc = self.load_vec("p_sc", psc_ap, KC)
            pw = c.sbuf("p_w", [128, KC, 256], BF16)
            self.load_w(pw, lambda kc, cs, cn: pw[:, kc, cs:cs + cn], pw_d, 0, KC, 0, 256)
            inv0 = c.sbuf("p_inv0", [128, 4, T], F32)
            c.ld("sp", inv0[:, :, :], inv_d[:, :, :], [inv_d], [inv0])
            sqb = [c.sbuf(f"p_sq{i}", [128, T], BF16) for i in range(2)]
            rs = c.sbuf("p_rs", [128, T], F32)
            xt = [c.sbuf(f"p_x{i}", [128, KC, HL + T], F32) for i in range(2)]
            hn = c.sbuf("p_hn", [128, KC, HL + T], F32)
            sa = c.sbuf("p_sa", [128, KC, HL + T], F32)
            sb = c.sbuf("p_sb", [128, KC, HL + T], F32)
            pp = c.sbuf("p_p", [128, KC, T], BF16)
            yt = c.sbuf("p_y", [128, T], F32)
            ps = [c.psum(f"p_ps{i}", [128, 512]) for i in range(4)]
            W = HL + T
            ntile = S // T
            for it, j in enumerate(reversed(range(ntile))):
                x = xt[it % 2]
                if j == 0:
                    c.call("dve", "memset", [], [x], x[:, :, 0:HL], 0.0)
                    for k in range(KC):
                        c.ld("sp", x[:, k, HL:W], xs[k * 128:(k + 1) * 128, 0:T], [(xs, (k, 0))], [x])
                else:
                    for k in range(KC):
                        c.ld("sp", x[:, k, :], xs[k * 128:(k + 1) * 128, j * T - HL:(j + 1) * T],
                             [(xs, (k, j)), (xs, (k, j - 1))], [x])
                self.norm_cols(x, hn, g, HL, slice(0, HL), slice(0, HL), ps[0], sqb, rs)
                self.norm_cols(x, hn, g, T, slice(HL, W), slice(HL, W), ps[1], sqb, rs)
                eng = "pool"
                c.call(eng, "tensor_tensor", [hn], [sa], out=sa[:, :, 1:W], in0=hn[:, :, 1:W], in1=hn[:, :, 0:W - 1], op=ALU.add)
                cur = {2: sa}
                c.call(eng, "tensor_tensor", [sa], [sb], out=sb[:, 2:, 3:W], in0=sa[:, 2:, 3:W], in1=sa[:, 2:, 1:W - 2], op=ALU.add)
                s8 = c.sbuf(f"p_s8_{it}", [128, 4, HL + T], F32) if it == 0 else self._s8
                self._s8 = s8
                c.call(eng, "tensor_tensor", [sb], [s8], out=s8[:, :, 7:W], in0=sb[:, 4:, 7:W], in1=sb[:, 4:, 3:W - 4], op=ALU.add)
                s16 = c.sbuf(f"p_s16_{it}", [128, 2, HL + T], F32) if it == 0 else self._s16
                self._s16 = s16
                c.call(eng, "tensor_tensor", [s8], [s16], out=s16[:, :, 15:W], in0=s8[:, 2:, 15:W], in1=s8[:, 2:, 7:W - 8], op=ALU.add)
                srcs = [(sa, 0), (sb, 2), (s8, 0), (s16, 0)]
                for gi in range(4):
                    st_, off = srcs[gi]
                    for kk in range(2):
                        k = gi * 2 + kk
                        if j == 0:
                            c.call("dve", "tensor_tensor", [st_, inv0], [yt], out=yt[:, :], in0=st_[:, off + kk, HL:W],
                                   in1=inv0[:, gi, :], op=ALU.mult)
                            c.call("dve", "tensor_tensor", [yt, hn], [(pp, k)], out=pp[:, k, :], in0=yt[:, :],
                                   in1=hn[:, k, HL:W], op=ALU.subtract)
                        else:
                            c.call("dve", "scalar_tensor_tensor", [st_, hn], [(pp, k)], out=pp[:, k, :],
                                   in0=st_[:, off + kk, HL:W], scalar=1.0 / WINS[gi], in1=hn[:, k, HL:W],
                                   op0=ALU.mult, op1=ALU.subtract)
                for oc in range(KC):
                    gi = oc // 2
                    p = ps[2 + oc % 2]
                    for kk in range(2):
                        c.mm(p[:, :], pw[:, gi * 2 + kk, (oc % 2) * 128:(oc % 2 + 1) * 128], pp[:, gi * 2 + kk, :], kk == 0, kk == 1,
                             [pw, (pp, gi * 2 + kk)], [p])
                    c.call("dve", "tensor_scalar", [p, pb, psc], [yt], out=yt[:, :], in0=p[:, :], scalar1=pb[:, oc:oc + 1],
                           scalar2=psc[:, oc:oc + 1], op0=ALU.add, op1=ALU.mult)
                    c.call("dve", "tensor_tensor", [yt, x], [x], out=x[:, oc, HL:W], in0=yt[:, :], in1=x[:, oc, HL:W], op=ALU.add)
                    c.ld("sp", xs[oc * 128:(oc + 1) * 128, j * T:(j + 1) * T], x[:, oc, HL:W], [x], [(xs, (oc, j))])

    def conv(self, xs, g_ap, win_d, bin_ap, dw_d, bdw_ap, lng_ap, lnb_ap, wout_d, bout_ap, uT):
        c, nc, S = self.c, self.nc, self.S
        T = 512
        HL = 32
        with c.phase():
            g = self.load_vec("c_g", g_ap, KC)
            b_in = self.load_vec("c_bin", bin_ap, 2 * KC)
            win = c.sbuf("c_win", [128, KC, 2048], BF16)
            self.load_w(win, lambda kc, cs, cn: win[:, kc, cs:cs + cn], win_d, 0, KC, 0, 2048)
            sqb = [c.sbuf(f"c_sq{i}", [128, T], BF16) for i in range(2)]
            rs = c.sbuf("c_rs", [128, T], F32)
            xt = [c.sbuf(f"c_x{i}", [128, KC, T], F32) for i in range(2)]
            xn = c.sbuf("c_xn", [128, KC, T], BF16)
            sg = [c.sbuf(f"c_sg{i}", [128, T], F32) for i in range(2)]
            ut = [c.sbuf(f"c_u{i}", [128, KC, T], F32) for i in range(2)]
            zt = c.sbuf("c_z", [128, HL], F32)
            ps = [c.psum(f"c_ps{i}", [128, 512]) for i in range(8)]
            pi = [0]

            def nps():
                pi[0] += 1
                return ps[pi[0] % 8]
            c.call("dve", "memset", [], [zt], zt[:, :], 0.0)
            for k in range(KC):
                c.ld("sp", uT[k * 128:(k + 1) * 128, 0:HL], zt[:, :], [zt], [(uT, (k, -1))])
            for tg in range(S // T):
                x, u = xt[tg % 2], ut[tg % 2]
                cs = slice(tg * T, (tg + 1) * T)
                for k in range(KC):
                    c.ld("sp", x[:, k, :], xs[k * 128:(k + 1) * 128, cs], [(xs, (k, tg))], [(x, k)])
                self.norm_cols(x, xn, g, T, slice(0, T), slice(0, T), nps(), sqb, rs)
                for oc in range(KC):
                    pa, pg = nps(), nps()
                    for k in range(KC):
                        c.mm(pa[:, :], win[:, k, oc * 128:(oc + 1) * 128], xn[:, k, :], k == 0, k == KC - 1, [win, xn], [pa])
                    for k in range(KC):
                        c.mm(pg[:, :], win[:, k, D + oc * 128:D + (oc + 1) * 128], xn[:, k, :], k == 0, k == KC - 1, [win, xn], [pg])
                    s = sg[oc % 2]
                    c.act(s[:, :], pg[:, :], AF.Sigmoid, [pg, b_in], [s], bias=b_in[:, KC + oc:KC + oc + 1])
                    c.call("dve", "scalar_tensor_tensor", [pa, b_in, s], [(u, oc)], out=u[:, oc, :], in0=pa[:, :],
                           scalar=b_in[:, oc:oc + 1], in1=s[:, :], op0=ALU.add, op1=ALU.mult)
                    c.ld("sp", uT[oc * 128:(oc + 1) * 128, HL + tg * T: HL + (tg + 1) * T], u[:, oc, :], [(u, oc)], [(uT, (oc, tg))])
        with c.phase():
            dw = c.sbuf("c_dw", [128, KC, 31], F32)
            c.ld("sp", dw[:, :, :], dw_d[:, :, :], [dw_d], [dw])
            bdw = self.load_vec("c_bdw", bdw_ap, KC)
            lng = self.load_vec("c_lng", lng_ap, KC)
            lnb = self.load_vec("c_lnb", lnb_ap, KC)
            bout = self.load_vec("c_bout", bout_ap, KC)
            wout = c.sbuf("c_wout", [128, KC, D], BF16)
            self.load_w(wout, lambda kc, cs, cn: wout[:, kc, cs:cs + cn], wout_d, 0, KC, 0, D)
            uh = [c.sbuf(f"c_uh{i}", [128, KC, HL + T], F32) for i in range(2)]
            v = c.sbuf("c_v", [128, KC, T], F32)
            vb = [c.sbuf(f"c_vb{i}", [128, T], BF16) for i in range(2)]
            vq = [c.sbuf(f"c_vq{i}", [128, T], BF16) for i in range(2)]
            mean = c.sbuf("c_mean", [128, T], F32)
            var = c.sbuf("c_var", [128, T], F32)
            t1 = [c.sbuf(f"c_t1{i}", [128, T], F32) for i in range(2)]
            sv = c.sbuf("c_sv", [128, KC, T], BF16)
            xt = [c.sbuf(f"c_xx{i}", [128, KC, T], F32) for i in range(2)]
            yt = c.sbuf("c_yt", [128, T], F32)
            ps = [c.psum(f"c_qs{i}", [128, 512]) for i in range(6)]
            for tg in range(S // T):
                uu, x = uh[tg % 2], xt[tg % 2]
                cs = slice(tg * T, (tg + 1) * T)
                for k in range(KC):
                    c.ld("sp", uu[:, k, :], uT[k * 128:(k + 1) * 128, tg * T: tg * T + HL + T],
                         [(uT, (k, tg)), (uT, (k, tg - 1))], [(uu, k)])
                    c.ld("sp", x[:, k, :], xs[k * 128:(k + 1) * 128, cs], [(xs, (k, tg))], [(x, k)])
                for k0 in range(0, KC, 2):
                    for j in range(31):
                        for k in (k0, k0 + 1):
                            if j == 0:
                                c.call("dve", "tensor_scalar", [(uu, k), dw, bdw], [(v, k)], out=v[:, k, :], in0=uu[:, k, 2:2 + T],
                                       scalar1=dw[:, k, 0:1], scalar2=bdw[:, k:k + 1], op0=ALU.mult, op1=ALU.add)
                            else:
                                c.call("dve", "scalar_tensor_tensor", [(uu, k), dw, (v, k)], [(v, k)], out=v[:, k, :],
                                       in0=uu[:, k, 2 + j:2 + j + T], scalar=dw[:, k, j:j + 1], in1=v[:, k, :], op0=ALU.mult, op1=ALU.add)
                pm, pq = ps[0], ps[1]
                for k in range(KC):
                    c.act(vb[k % 2][:, :], v[:, k, :], AF.Copy, [(v, k)], [vb[k % 2]])
                    c.act(vq[k % 2][:, :], v[:, k, :], AF.Square, [(v, k)], [vq[k % 2]])
                    c.mm(pm[:, :], self.onesb[:, :], vb[k % 2][:, :], k == 0, k == KC - 1, [self.onesb, vb[k % 2]], [pm])
                    c.mm(pq[:, :], self.onesb[:, :], vq[k % 2][:, :], k == 0, k == KC - 1, [self.onesb, vq[k % 2]], [pq])
                c.act(mean[:, :], pm[:, :], AF.Copy, [pm], [mean], scale=1.0 / D)
                c.call("dve", "tensor_tensor", [mean], [var], out=var[:, :], in0=mean[:, :], in1=mean[:, :], op=ALU.mult)
                c.call("dve", "scalar_tensor_tensor", [pq, var], [var], out=var[:, :], in0=pq[:, :], scalar=1.0 / D, in1=var[:, :],
                       op0=ALU.mult, op1=ALU.subtract)
                c.act(var[:, :], var[:, :], AF.Sqrt, [var, self.eps], [var], bias=self.eps[:, 0:1], scale=1.0)
                c.call("dve", "reciprocal", [var], [var], out=var[:, :], in_=var[:, :])
                for k in range(KC):
                    t = t1[k % 2]
                    c.call("dve", "tensor_tensor", [(v, k), mean], [t], out=t[:, :], in0=v[:, k, :], in1=mean[:, :], op=ALU.subtract)
                    c.call("dve", "tensor_tensor", [t, var], [t], out=t[:, :], in0=t[:, :], in1=var[:, :], op=ALU.mult)
                    c.act(sv[:, k, :], t[:, :], AF.Silu, [t, lng, lnb], [(sv, k)], scale=lng[:, k:k + 1], bias=lnb[:, k:k + 1])
                for dc in range(KC):
                    p = ps[2 + dc % 4]
                    for k in range(KC):
                        c.mm(p[:, :], wout[:, k, dc * 128:(dc + 1) * 128], sv[:, k, :], k == 0, k == KC - 1, [wout, (sv, k)], [p])
                    c.call("dve", "scalar_tensor_tensor", [p, bout, (x, dc)], [(x, dc)], out=x[:, dc, :], in0=p[:, :],
                           scalar=bout[:, dc:dc + 1], in1=x[:, dc, :], op0=ALU.add, op1=ALU.add)
                    c.ld("sp", xs[dc * 128:(dc + 1) * 128, cs], x[:, dc, :], [(x, dc)], [(xs, (dc, tg))])


def _gla(self, xs, g_ap, win_d, wgu_d, bg_ap, ng_ap, wo_d, cst):
    c, nc, S = self.c, self.nc, self.S
    T = 512
    with c.phase():
        g = self.load_vec("g_g", g_ap, KC)
        bg = self.load_vec("g_bg", bg_ap, 4)
        ng = self.load_vec("g_ng", ng_ap, 2)
        nbg = c.sbuf("g_nbg", [128, 4], F32)
        c.call("dve", "tensor_scalar", [bg], [nbg], out=nbg[:, :], in0=bg[:, :], scalar1=-1.0, scalar2=None, op0=ALU.mult)
        win = c.sbuf("g_win", [128, KC, 3088], BF16)
        self.load_w(win, lambda kc, cs, cn: win[:, kc, cs:cs + cn], win_d, 0, KC, 0, 3088)
        wo = c.sbuf("g_wo", [128, KC, D], BF16)
        self.load_w(wo, lambda kc, cs, cn: wo[:, kc, cs:cs + cn], wo_d, 0, KC, 0, D)
        wgu = c.sbuf("g_wgu", [16, 512], BF16)
        st = self.stg[0]
        c.ld("sp", st[0:16, 0:512], wgu_d[:, :], [wgu_d], [st])
        c.call("pool", "tensor_copy", [st], [wgu], out=wgu[:, :], in_=st[0:16, 0:512])
        smask = c.sbuf("g_smask", [128, T], F32)
        c.ld("sp", smask[:, :], cst["scanmask"][:, :], [], [smask])
        gmask = c.sbuf("g_gmask", [128, 128], F32)
        c.ld("sp", gmask[:, :], cst["gmask"][:, :], [], [gmask])
        ident = c.sbuf("g_ident", [128, 128], BF16)
        c.ld("sp", ident[:, :], cst["ident"][:, :], [], [ident])
        sqb = [c.sbuf(f"g_sq{i}", [128, T], BF16) for i in range(2)]
        rs = c.sbuf("g_rs", [128, T], F32)
        xt = [c.sbuf(f"g_x{i}", [128, KC, T], F32) for i in range(1)]
        xn = c.sbuf("g_xn", [128, KC, T], BF16)
        gdT = c.sbuf("g_gd", [16, T], BF16)
        bT = c.sbuf("g_bT", [128, 4, T], F32)
        e1 = [c.sbuf(f"g_e{i}", [128, T], F32) for i in range(2)]
        nbl = c.sbuf("g_nbl", [128, 4, 8], F32)
        dec = c.sbuf("g_dec", [128, 4, 8], F32)
        qt_ = c.sbuf("g_qt", [128, 4, T], BF16)
        kt_ = c.sbuf("g_kt", [128, 4, T], BF16)
        kdT = c.sbuf("g_kdT", [128, 4, T], BF16)
        kd = c.sbuf("g_kd", [128, 4, 4, 128], BF16)
        vtok = c.sbuf("g_vtok", [128, 4, D], BF16)
        sog = c.sbuf("g_sog", [128, KC, T], BF16)
        o = c.sbuf("g_o", [128, KC, T], F32)
        on = o
        ob = c.sbuf("g_ob", [128, KC, T], BF16)
        At = [c.sbuf(f"g_At{i}", [128, 128], BF16) for i in range(2)]
        Sf = c.sbuf("g_S", [128, 4, 256], F32)
        Sb = [c.sbuf(f"g_Sb{i}", [128, 4, 256], BF16) for i in range(2)]
        c.call("dve", "memset", [], [Sf], Sf[:, :, :], 0.0)
        c.call("dve", "memset", [], [Sb[0]], Sb[0][:, :, :], 0.0)
        ps = [c.psum(f"g_ps{i}", [128, 512]) for i in range(7)]
        ptr = c.psum("g_ptr", [128, 4, 128], BF16)
        pi = [0]

        def nps():
            pi[0] += 1
            return ps[pi[0] % 7]
        nchunk = 0
        for tg in range(S // T):
            x = xt[0]
            cs = slice(tg * T, (tg + 1) * T)
            for k in range(KC):
                c.ld("sp", x[:, k, :], xs[k * 128:(k + 1) * 128, cs], [(xs, (k, tg))], [(x, k)])
            self.norm_cols(x, xn, g, T, slice(0, T), slice(0, T), nps(), sqb, rs)
            p = nps()
            for k in range(KC):
                c.mm(p[0:16, :], win[:, k, 3072:3088], xn[:, k, :], k == 0, k == KC - 1, [win, xn], [p])
            c.call("dve", "tensor_copy", [p], [gdT], out=gdT[:, :], in_=p[0:16, :])
            for h in range(4):
                p = nps()
                c.mm(p[:, :], wgu[:, h * 128:(h + 1) * 128], gdT[:, :], True, True, [wgu, gdT], [p])
                e = e1[h % 2]
                c.act(e[:, :], p[:, :], AF.Exp, [p, nbg], [e], scale=-1.0, bias=nbg[:, h:h + 1])
                c.act(e[:, :], e[:, :], AF.Ln, [e], [e], bias=1.0, scale=1.0)
                c.call("dve", "tensor_tensor_scan", [smask, e], [(bT, h)], out=bT[:, h, :], data0=smask[:, :], data1=e[:, :],
                       initial=0.0, op0=ALU.mult, op1=ALU.add)
                c.call("dve", "tensor_scalar", [(bT, h)], [(nbl, h)], out=nbl[:, h, :], in0=bT[:, h, 63::64], scalar1=-1.0 / 16,
                       scalar2=None, op0=ALU.mult)
                c.act(dec[:, h, :], nbl[:, h, :], AF.Exp, [(nbl, h)], [(dec, h)])
                p = nps()
                for k in range(KC):
                    c.mm(p[:, :], win[:, k, h * 128:(h + 1) * 128], xn[:, k, :], k == 0, k == KC - 1, [win, xn], [p])
                e = e1[(h + 1) % 2]
                c.act(e[:, :], bT[:, h, :], AF.Exp, [(bT, h)], [e], scale=-1.0 / 16)
                c.call("dve", "scalar_tensor_tensor", [p, e], [(qt_, h)], out=qt_[:, h, :], in0=p[:, :], scalar=128 ** -0.5,
                       in1=e[:, :], op0=ALU.mult, op1=ALU.mult)
                p = nps()
                for k in range(KC):
                    c.mm(p[:, :], win[:, k, 512 + h * 128:512 + (h + 1) * 128], xn[:, k, :], k == 0, k == KC - 1, [win, xn], [p])
                e = e1[h % 2]
                c.act(e[:, :], bT[:, h, :], AF.Exp, [(bT, h)], [e], scale=1.0 / 16)
                c.call("dve", "tensor_tensor", [p, e], [(kt_, h)], out=kt_[:, h, :], in0=p[:, :], in1=e[:, :], op=ALU.mult)
                for n in range(8):
                    c.act(e[:, n * 64:(n + 1) * 64], bT[:, h, n * 64:(n + 1) * 64], AF.Exp, [(bT, h), (nbl, h)], [e],
                          scale=1.0 / 16, bias=nbl[:, h, n:n + 1])
                c.call("dve", "tensor_tensor", [p, e], [(kdT, h)], out=kdT[:, h, :], in0=p[:, :], in1=e[:, :], op=ALU.mult)
                for tt in range(4):
                    c.op("pe", (lambda o_=ptr[:, tt, :], i_=kdT[:, h, tt * 128:(tt + 1) * 128], id_=ident[:, :]:
                                nc.tensor.transpose(o_, i_, id_)), reads=[(kdT, h), ident], writes=[ptr])
                c.call("dve", "tensor_copy", [ptr], [(kd, h)], out=kd[:, h, :, :], in_=ptr[:, :, :])
            for tt in range(4):
                for nb in range(2):
                    p = nps()
                    for k in range(KC):
                        c.mm(p[:, :], xn[:, k, tt * 128:(tt + 1) * 128], win[:, k, 1024 + nb * 512:1024 + (nb + 1) * 512],
                             k == 0, k == KC - 1, [win, xn], [p])
                    c.call("dve", "tensor_copy", [p], [(vtok, (tt, nb))], out=vtok[:, tt, nb * 512:(nb + 1) * 512], in_=p[:, :])
            for oc in range(KC):
                p = nps()
                for k in range(KC):
                    c.mm(p[:, :], win[:, k, 2048 + oc * 128:2048 + (oc + 1) * 128], xn[:, k, :], k == 0, k == KC - 1, [win, xn], [p])
                c.act(sog[:, oc, :], p[:, :], AF.Silu, [p], [(sog, oc)])
            for tt in range(4):
                for h in range(4):
                    pc = slice(tt * 128, (tt + 1) * 128)
                    pa = nps()
                    c.mm(pa[:, 0:128], kt_[:, h, pc], qt_[:, h, pc], True, True, [(kt_, h), (qt_, h)], [pa])
                    A = At[(tt * 4 + h) % 2]
                    c.call("dve", "tensor_tensor", [pa, gmask], [A], out=A[:, :], in0=pa[:, 0:128], in1=gmask[:, :], op=ALU.mult)
                    n0 = nchunk + 2 * tt
                    po = [nps(), nps()]
                    for mc in range(2):
                        c.mm(po[mc][:, 0:128], vtok[:, tt, h * 256 + mc * 128:h * 256 + (mc + 1) * 128], A[:, :], True, False,
                             [vtok, A], [po[mc]])
                        c.mm(po[mc][:, 0:64], Sb[0][:, h, mc * 128:(mc + 1) * 128], qt_[:, h, tt * 128:tt * 128 + 64], False, False,
                             [(Sb[0], h), (qt_, h)], [po[mc]])
                    pk = nps()
                    c.mm(pk[:, 0:256], kd[0:64, h, tt, :], vtok[0:64, tt, h * 256:(h + 1) * 256], True, True, [(kd, h), vtok], [pk])
                    c.call("dve", "scalar_tensor_tensor", [pk, (Sf, h), (dec, h)], [(Sf, h)], out=Sf[:, h, :], in0=Sf[:, h, :],
                           scalar=dec[:, h, 2 * tt:2 * tt + 1], in1=pk[:, 0:256], op0=ALU.mult, op1=ALU.add)
                    c.act(Sb[1][:, h, :], Sf[:, h, :], AF.Copy, [(Sf, h)], [(Sb[1], h)])
                    for mc in range(2):
                        c.mm(po[mc][:, 64:128], Sb[1][:, h, mc * 128:(mc + 1) * 128], qt_[:, h, tt * 128 + 64:(tt + 1) * 128], False, True,
                             [(Sb[1], h), (qt_, h)], [po[mc]])
                        c.act(o[:, h * 2 + mc, pc], po[mc][:, 0:128], AF.Copy, [po[mc]], [(o, (h, mc, tt))])
                    pk = nps()
                    c.mm(pk[:, 0:256], kd[64:128, h, tt, :], vtok[64:128, tt, h * 256:(h + 1) * 256], True, True, [(kd, h), vtok], [pk])
                    c.call("dve", "scalar_tensor_tensor", [pk, (Sf, h), (dec, h)], [(Sf, h)], out=Sf[:, h, :], in0=Sf[:, h, :],
                           scalar=dec[:, h, 2 * tt + 1:2 * tt + 2], in1=pk[:, 0:256], op0=ALU.mult, op1=ALU.add)
                    c.act(Sb[0][:, h, :], Sf[:, h, :], AF.Copy, [(Sf, h)], [(Sb[0], h)])
            nchunk += 8
            for h in range(4):
                class _V:
                    pass
                for kk in range(2):
                    s = sqb[kk % 2]
                    c.act(s[:, :], o[:, 2 * h + kk, :], AF.Square, [o], [s])
                    pn = ps[0] if kk == 0 else pn
                    c.mm(pn[:, :], self.onesb[:, :], s[:, :], kk == 0, kk == 1, [self.onesb, s], [pn])
                c.act(rs[:, :], pn[:, :], AF.Sqrt, [pn, self.eps], [rs], bias=self.eps[:, 0:1], scale=1.0 / 256)
                c.call("dve", "reciprocal", [rs], [rs], out=rs[:, :], in_=rs[:, :])
                for kk in range(2):
                    k = 2 * h + kk
                    c.call("dve", "scalar_tensor_tensor", [o, ng, rs], [(on, k)], out=on[:, k, :], in0=o[:, k, :], scalar=ng[:, kk:kk + 1],
                           in1=rs[:, :], op0=ALU.mult, op1=ALU.mult)
                    c.call("dve", "tensor_tensor", [(on, k), (sog, k)], [(ob, k)], out=ob[:, k, :], in0=on[:, k, :], in1=sog[:, k, :], op=ALU.mult)
            for dc in range(KC):
                p = nps()
                for k in range(KC):
                    c.mm(p[:, :], wo[:, k, dc * 128:(dc + 1) * 128], ob[:, k, :], k == 0, k == KC - 1, [wo, (ob, k)], [p])
                c.call("dve", "tensor_tensor", [p, (x, dc)], [(x, dc)], out=x[:, dc, :], in0=p[:, :], in1=x[:, dc, :], op=ALU.add)
                c.ld("sp", xs[dc * 128:(dc + 1) * 128, cs], x[:, dc, :], [(x, dc)], [(xs, (dc, tg))])


M.gla = _gla


def _nsa(self, xs, g_ap, dr, cst):
    c, nc, S = self.c, self.nc, self.S
    T = 512
    NQ, NKC, NCMP, NCP = S // T, S // 128, S // 16 - 1, S // 16
    NC2 = NCP // 128
    if not hasattr(self, "_nsa_d"):
        self._nsa_d = dict(qT=c.dram("n_qT", [1024, S], BF16), kvT=c.dram("n_kvT", [1536, S], BF16),
                           vtok=c.dram("n_vtok", [S, 512], BF16), gT=c.dram("n_gT", [48, S], F32),
                           oT=c.dram("n_oT", [1024, S], BF16))
    qT_d, kvT_d, vtok_d, gT_d, oT_d = (self._nsa_d[k] for k in ("qT", "kvT", "vtok", "gT", "oT"))
    with c.phase():
        g = self.load_vec("s_g", g_ap, KC)
        win = c.sbuf("s_win", [128, KC, 2608], BF16)
        self.load_w(win, lambda kc, cs, cn: win[:, kc, cs:cs + cn], dr["nsa_w_in"], 0, KC, 0, 2608)
        sqb = [c.sbuf(f"s_sq{i}", [128, T], BF16) for i in range(2)]
        rs = c.sbuf("s_rs", [128, T], F32)
        xt = [c.sbuf(f"s_x{i}", [128, KC, T], F32) for i in range(2)]
        xn = c.sbuf("s_xn", [128, KC, T], BF16)
        qo = [c.sbuf(f"s_qo{i}", [128, T], BF16) for i in range(3)]
        go = c.sbuf("s_go", [48, T], F32)
        ps = [c.psum(f"s_ps{i}", [128, 512]) for i in range(8)]
        pi = [0]

        def nps():
            pi[0] += 1
            return ps[pi[0] % 8]
        qi = 0
        for tg in range(NQ):
            x = xt[tg % 2]
            cs = slice(tg * T, (tg + 1) * T)
            for k in range(KC):
                c.ld("sp", x[:, k, :], xs[k * 128:(k + 1) * 128, cs], [(xs, (k, tg))], [(x, k)])
            self.norm_cols(x, xn, g, T, slice(0, T), slice(0, T), nps(), sqb, rs)
            for oc in range(20):
                p = nps()
                for k in range(KC):
                    c.mm(p[:, :], win[:, k, oc * 128:(oc + 1) * 128], xn[:, k, :], k == 0, k == KC - 1, [win, xn], [p])
                q = qo[qi % 3]
                qi += 1
                c.act(q[:, :], p[:, :], AF.Copy, [p], [q], scale=(0.125 if oc < 8 else 1.0))
                if oc < 8:
                    c.ld("sp", qT_d[oc * 128:(oc + 1) * 128, cs], q[:, :], [q], [(qT_d, (oc, tg))])
                else:
                    c.ld("sp", kvT_d[(oc - 8) * 128:(oc - 7) * 128, cs], q[:, :], [q], [(kvT_d, (oc, tg))])
            p = nps()
            for k in range(KC):
                c.mm(p[0:48, :], win[:, k, 2560:2608], xn[:, k, :], k == 0, k == KC - 1, [win, xn], [p])
            c.act(go[:, :], p[0:48, :], AF.Sigmoid, [p], [go])
            c.ld("sp", gT_d[:, cs], go[:, :], [go], [(gT_d, tg)])
            for tt in range(4):
                p = nps()
                for half, c0 in enumerate((1792, 2304)):
                    for k in range(KC):
                        c.mm(p[:, half * 256:(half + 1) * 256], xn[:, k, tt * 128:(tt + 1) * 128], win[:, k, c0:c0 + 256],
                             k == 0, k == KC - 1, [win, xn], [p])
                q = qo[qi % 3]
                qi += 1
                c.call("dve", "tensor_copy", [p], [q], out=q[:, :], in_=p[:, :])
                c.ld("sp", vtok_d[tg * T + tt * 128: tg * T + (tt + 1) * 128, :], q[:, :], [q], [(vtok_d, (tg, tt))])
    with c.phase():
        stg0 = self.stg[0]
        kcmpT = c.sbuf("s_kcmpT", [128, 4, NCP], BF16)
        vcmp = c.sbuf("s_vcmp", [128, 4, NC2, 64], BF16)
        with c.phase():
            w1b = c.sbuf("s_w1b", [64, 32, 256], BF16)
            posf = c.sbuf("s_posf", [64, 32], F32)
            posb = c.sbuf("s_posb", [64, 32], BF16)
            c1 = c.sbuf("s_c1", [128, 2], F32)
            w2f = c.sbuf("s_w2f", [128, 2, 64], F32)
            w2b = c.sbuf("s_w2b", [128, 2, 128], BF16)
            hb = c.sbuf("s_hb", [128, 2, NCP], BF16)
            aT = [c.sbuf(f"s_aT{i}", [64, S], BF16) for i in range(2)]
            xg = c.sbuf("s_xg", [128, NCP], F32)
            t1 = c.sbuf("s_t1", [128, NCP], F32)
            t2 = c.sbuf("s_t2", [128, NCP], F32)
            ps = [c.psum(f"s_cs{i}", [128, 512]) for i in range(4)]
            pi = [0]

            def nps():
                pi[0] += 1
                return ps[pi[0] % 4]
            c.call("dve", "memset", [], [hb], hb[:, :, :], 0.0)
            c.call("dve", "memset", [], [w2b], w2b[:, :, :], 0.0)
            for typ in range(2):
                for j in range(32):
                    st = self.stg[self._stg_i % 4]
                    self._stg_i += 1
                    c.ld("sp", st[0:64, 0:256], dr["nsa_cmp_w1"].t[typ, j * 64:(j + 1) * 64, :], [], [st])
                    c.call("pool", "tensor_copy", [st], [(w1b, j)], out=w1b[:, j, :], in_=st[0:64, 0:256])
                c.ld("sp", posf[:, :], dr["nsa_cmp_pos"].t[typ].rearrange("j d -> d j"), [], [posf], allow_slow_non_contiguous=True)
                c.call("dve", "tensor_copy", [posf], [posb], out=posb[:, :], in_=posf[:, :])
                c.ld("sp", w2f[:, :, :], dr["nsa_cmp_w2"].t[typ].rearrange("(c p) d -> p c d", p=128), [], [w2f])
                c.call("dve", "tensor_copy", [w2f], [w2b], out=w2b[:, :, 64:128], in_=w2f[:, :, :])
                for hc in range(2):
                    p = nps()
                    for j in range(32):
                        c.mm(p[:, 0:1], w1b[:, j, hc * 128:(hc + 1) * 128], posb[:, j:j + 1], j == 0, j == 31, [w1b, posb], [p])
                    c.call("dve", "tensor_copy", [p], [(c1, hc)], out=c1[:, hc:hc + 1], in_=p[:, 0:1])
                for gq in range(4):
                    a = aT[gq % 2]
                    c.ld("sp", a[:, :], kvT_d[typ * 256 + gq * 64: typ * 256 + (gq + 1) * 64, :], [kvT_d], [a])
                    for hc in range(2):
                        for n0 in range(0, NCMP, 256):
                            nn = min(256, NCMP - n0)
                            p = nps()
                            for j in range(32):
                                c.mm(p[:, 0:nn], w1b[:, j, hc * 128:(hc + 1) * 128],
                                     a[:, 16 * n0 + j: 16 * n0 + j + 16 * (nn - 1) + 1: 16], j == 0, j == 31, [w1b, a], [p])
                            ns = slice(n0, n0 + nn)
                            c.call("dve", "tensor_scalar", [p, c1], [xg], out=xg[:, ns], in0=p[:, 0:nn], scalar1=c1[:, hc:hc + 1],
                                   scalar2=None, op0=ALU.add)
                            c.act(t1[:, ns], xg[:, ns], AF.Square, [xg], [t1])
                            c.call("dve", "tensor_scalar", [t1], [t1], out=t1[:, ns], in0=t1[:, ns], scalar1=0.044715, scalar2=1.0,
                                   op0=ALU.mult, op1=ALU.add)
                            c.call("dve", "tensor_tensor", [t1, xg], [t2], out=t2[:, ns], in0=t1[:, ns], in1=xg[:, ns], op=ALU.mult)
                            c.act(t2[:, ns], t2[:, ns], AF.Sigmoid, [t2], [t2], scale=1.5957691216)
                            c.call("dve", "tensor_tensor", [t2, xg], [hb], out=hb[:, hc, ns], in0=t2[:, ns], in1=xg[:, ns], op=ALU.mult)
                    if typ == 0:
                        for n0 in range(0, NCMP, 512):
                            nn = min(512, NCMP - n0)
                            p = nps()
                            for hc in range(2):
                                c.mm(p[:, 0:nn], w2b[:, hc, :], hb[:, hc, n0:n0 + nn], hc == 0, hc == 1, [w2b, hb], [p])
                            c.call("dve", "tensor_copy", [p], [(kcmpT, gq)], out=kcmpT[64:128, gq, n0:n0 + nn], in_=p[64:128, 0:nn])
                        c.call("dve", "memset", [], [(kcmpT, gq)], kcmpT[64:128, gq, NCMP:NCP], 0.0)
                    else:
                        for ncx in range(NC2):
                            p = nps()
                            for hc in range(2):
                                c.mm(p[:, 0:64], hb[:, hc, ncx * 128:(ncx + 1) * 128], w2b[:, hc, 64:128], hc == 0, hc == 1, [w2b, hb], [p])
                            c.call("dve", "tensor_copy", [p], [(vcmp, gq)], out=vcmp[:, gq, ncx, :], in_=p[:, 0:64])
        KS = c.sbuf("s_KS", [128, S], BF16)
        KW = c.sbuf("s_KW", [128, S], BF16)
        VS = c.sbuf("s_VS", [128, NKC, 64], BF16)
        VW = c.sbuf("s_VW", [128, NKC, 64], BF16)
        QS = [c.sbuf(f"s_QS{r}", [128, S], BF16) for r in range(4)]
        vis = c.sbuf("s_vis", [128, NC2, S], BF16)
        wm = c.sbuf("s_wm", [128, 8, T], BF16)
        Fc = c.sbuf("s_F", [128, NKC, 64], F32)
        Uc = c.sbuf("s_U", [128, NKC, 64], F32)
        ov = c.sbuf("s_ov", [128, NC2, 64], BF16)
        identf = c.sbuf("s_idf", [128, 128], F32)
        c.ld("sp", vis[:, :, :], cst["nsa_vis"][:, :, :], [], [vis])
        c.ld("sp", wm[:, :, :], cst["nsa_wm"][:, :, :], [], [wm])
        c.ld("sp", Fc[:, :, :], cst["nsa_F"][:, :, :], [], [Fc])
        c.ld("sp", Uc[:, :, :], cst["nsa_U"][:, :, :], [], [Uc])
        c.ld("sp", ov[:, :, :], cst["nsa_ov"][:, :, :], [], [ov])
        c.ld("sp", identf[:, :], cst["identf"][:, :], [], [identf])
        c.ld("sp", KS[0:64, :], cst["nsa_E"][:, :], [], [(KS, "e")])
        Pc = c.sbuf("s_Pc", [128, 4, NC2, T], BF16)
        Pn = c.sbuf("s_Pn", [128, 4, NC2, T], BF16)
        ex = [c.sbuf(f"s_ex{i}", [128, T], BF16) for i in range(3)]
        Pt = [c.sbuf(f"s_P{i}", [128, T], BF16) for i in range(3)]
        rden = [c.sbuf(f"s_rden{i}", [128, T], F32) for i in range(2)]
        GB = [c.sbuf(f"s_GB{r}", [64, 3, T], F32) for r in range(4)]
        acc = [c.sbuf(f"s_acc{r}", [64, T], F32) for r in range(4)]
        tmp = [c.sbuf(f"s_tmp{i}", [64, T], F32) for i in range(2)]
        oh = [c.sbuf(f"s_oh{i}", [64, T], BF16) for i in range(2)]
        imp2 = c.sbuf("s_imp2", [128, 64], F32)
        impt = c.sbuf("s_impt", [128, 64], F32)
        m8 = c.sbuf("s_m8", [128, 16], F32)
        thr = c.sbuf("s_thr", [128, 1], F32)
        negm = c.sbuf("s_negm", [128, 64], F32)
        tiny = c.sbuf("s_tiny", [128, 1], F32)
        c.call("dve", "memset", [], [tiny], tiny[:, :], 1e-30)
        pst = [c.psum(f"s_st{i}", [128, 512]) for i in range(3)]
        pden = c.psum("s_pden", [128, 512])
        pov = c.psum("s_pov", [128, 512])
        pim = c.psum("s_pim", [128, 512])
        ptp = c.psum("s_ptp", [128, 512])
        cnt = [0]

        def nst():
            cnt[0] += 1
            return pst[cnt[0] % 3]

        def finish(r, br, first):
            rd = rden[cnt[0] % 2]
            c.act(rd[0:64, :], pden[0:64, :], AF.Ln, [pden, tiny], [rd], bias=tiny[0:64, 0:1], scale=1.0)
            c.act(rd[0:64, :], rd[0:64, :], AF.Exp, [rd], [rd], scale=-1.0)
            t = tmp[cnt[0] % 2]
            c.call("dve", "tensor_tensor", [pov, rd], [t], out=t[:, :], in0=pov[0:64, :], in1=rd[0:64, :], op=ALU.mult)
            if first:
                c.call("dve", "tensor_tensor", [t, (GB[r], br)], [acc[r]], out=acc[r][:, :], in0=t[:, :], in1=GB[r][:, br, :], op=ALU.mult)
            else:
                c.call("dve", "tensor_tensor", [t, (GB[r], br)], [t], out=t[:, :], in0=t[:, :], in1=GB[r][:, br, :], op=ALU.mult)
                if getattr(self, "dbg", None) is not None:
                    c.ld("sp", self.dbg[br, self._hd * 64:(self._hd + 1) * 64, self._qc], t[:, :], [t], [(self.dbg, (br, self._hd, self._qc.start))])
                c.call("dve", "tensor_tensor", [t, acc[r]], [acc[r]], out=acc[r][:, :], in0=t[:, :], in1=acc[r][:, :], op=ALU.add)

        for gq in range(4):
            c.ld("sp", KS[64:128, :], kvT_d[512 + gq * 64: 512 + (gq + 1) * 64, :], [kvT_d], [(KS, "k")])
            c.ld("sp", KW[64:128, :], kvT_d[1024 + gq * 64: 1024 + (gq + 1) * 64, :], [kvT_d], [KW])
            for cc in range(0, NKC, 8):
                c.ld("sp", VS[:, cc:cc + 8, :], vtok_d[cc * 128:(cc + 8) * 128, gq * 64:(gq + 1) * 64].rearrange("(c p) d -> p c d", p=128),
                     [vtok_d], [(VS, cc)])
                c.ld("sp", VW[:, cc:cc + 8, :], vtok_d[cc * 128:(cc + 8) * 128, 256 + gq * 64: 256 + (gq + 1) * 64].rearrange("(c p) d -> p c d", p=128),
                     [vtok_d], [(VW, cc)])
            for r in range(4):
                hd = gq * 4 + r
                c.ld("sp", QS[r][64:128, :], qT_d[hd * 64:(hd + 1) * 64, :], [qT_d], [(QS[r], "q")])
            for qt in range(NQ):
                qc = slice(qt * T, (qt + 1) * T)
                for r in range(4):
                    hd = gq * 4 + r
                    for br in range(3):
                        row = hd * 3 + br
                        c.ld("sp", GB[r][:, br, :], gT_d[row:row + 1, qc].to_broadcast([64, T]), [gT_d], [(GB[r], br)])
                for r in range(4):
                    for ncx in range(NC2):
                        p = nst()
                        c.mm(p[:, :], kcmpT[64:128, gq, ncx * 128:(ncx + 1) * 128], QS[r][64:128, qc], True, True,
                             [(kcmpT, gq), (QS[r], "q")], [p])
                        e = ex[cnt[0] % 3]
                        c.act(e[:, :], p[:, :], AF.Exp, [p], [e])
                        c.call("dve", "tensor_tensor", [e, vis], [(Pc, (r, ncx))], out=Pc[:, r, ncx, :], in0=e[:, :], in1=vis[:, ncx, qc], op=ALU.mult)
                    for ncx in range(NC2):
                        c.mm(pden[:, :], self.onesb[:, :], Pc[:, r, ncx, :], ncx == 0, ncx == NC2 - 1, [self.onesb, (Pc, (r, ncx))], [pden])
                    for ncx in range(NC2):
                        c.mm(pov[0:64, :], vcmp[:, gq, ncx, :], Pc[:, r, ncx, :], ncx == 0, ncx == NC2 - 1, [(vcmp, gq), (Pc, (r, ncx))], [pov])
                    cnt[0] += 1
                    rd = rden[cnt[0] % 2]
                    c.act(rd[:, :], pden[:, :], AF.Ln, [pden, tiny], [rd], bias=tiny[:, 0:1], scale=1.0)
                    c.act(rd[:, :], rd[:, :], AF.Exp, [rd], [rd], scale=-1.0)
                    t = tmp[cnt[0] % 2]
                    c.call("dve", "tensor_tensor", [pov, rd], [t], out=t[:, :], in0=pov[0:64, :], in1=rd[0:64, :], op=ALU.mult)
                    c.call("dve", "tensor_tensor", [t, (GB[r], 0)], [acc[r]], out=acc[r][:, :], in0=t[:, :], in1=GB[r][:, 0, :], op=ALU.mult)
                    if getattr(self, "dbg", None) is not None:
                        c.ld("sp", self.dbg[0, (gq * 4 + r) * 64:(gq * 4 + r + 1) * 64, qc], acc[r][:, :], [acc[r]], [(self.dbg, (0, gq * 4 + r, qt))])
                    for ncx in range(NC2):
                        c.call("pool", "tensor_tensor", [(Pc, (r, ncx)), rd], [(Pn, (r, ncx))], out=Pn[:, r, ncx, :], in0=Pc[:, r, ncx, :],
                               in1=rd[:, :], op=ALU.mult)
                for qs in range(4):
                    qq = slice(qs * 128, (qs + 1) * 128)
                    i = 0
                    for r in range(4):
                        for ncx in range(NC2):
                            c.mm(pim[:, 0:64], Pn[:, r, ncx, qq], ov[:, ncx, :], i == 0, i == 4 * NC2 - 1, [(Pn, (r, ncx)), ov], [pim])
                            i += 1
                    ti = qt * 4 + qs
                    c.call("dve", "tensor_tensor", [pim, Fc], [imp2], out=imp2[:, :], in0=pim[:, 0:64], in1=Fc[:, ti, :], op=ALU.max)
                    c.call("dve", "tensor_tensor", [imp2, Uc], [imp2], out=imp2[:, :], in0=imp2[:, :], in1=Uc[:, ti, :], op=ALU.min)
                    c.call("dve", "max", [imp2], [m8], out=m8[:, 0:8], in_=imp2[:, :])
                    c.call("dve", "match_replace", [imp2, m8], [impt], out=impt[:, :], in_to_replace=m8[:, 0:8], in_values=imp2[:, :], imm_value=-2.0)
                    c.call("dve", "max", [impt], [m8], out=m8[:, 8:16], in_=impt[:, :])
                    c.call("dve", "tensor_scalar", [m8], [thr], out=thr[:, :], in0=m8[:, 15:16], scalar1=-0.5, scalar2=None, op0=ALU.max)
                    c.call("dve", "tensor_scalar", [imp2, thr], [negm], out=negm[:, :], in0=imp2[:, :], scalar1=thr[:, 0:1], scalar2=1.0,
                           op0=ALU.is_ge, op1=ALU.subtract)
                    c.call("dve", "tensor_scalar", [negm], [negm], out=negm[:, :], in0=negm[:, :], scalar1=30000.0, scalar2=None, op0=ALU.mult)
                    c.op("pe", (lambda o_=ptp[0:64, 0:128], i_=negm[:, :], id_=identf[:, :]: nc.tensor.transpose(o_, i_, id_)),
                         reads=[negm, identf], writes=[ptp])
                    for r in range(4):
                        dst = QS[r][0:64, qt * T + qs * 128: qt * T + (qs + 1) * 128]
                        if r % 2 == 0:
                            c.act(dst, ptp[0:64, 0:128], AF.Copy, [ptp], [(QS[r], ("m", qt, qs))])
                        else:
                            c.call("dve", "tensor_copy", [ptp], [(QS[r], ("m", qt, qs))], out=dst, in_=ptp[0:64, 0:128])
                for r in range(4):
                    hd = gq * 4 + r
                    self._hd, self._qc = hd, qc
                    nk = 4 * (qt + 1)
                    for kc in range(nk):
                        p = nst()
                        c.mm(p[:, :], KS[:, kc * 128:(kc + 1) * 128], QS[r][:, qc], True, True,
                             [KS, (QS[r], "q")] + [(QS[r], ("m", qt, s_)) for s_ in range(4)], [p])
                        if kc >= 4 * qt:
                            e = ex[cnt[0] % 3]
                            c.act(e[:, :], p[:, :], AF.Exp, [p], [e])
                            P = Pt[cnt[0] % 3]
                            c.call("pool", "tensor_tensor", [e, wm], [P], out=P[:, :], in0=e[:, :], in1=wm[:, 4 + kc - 4 * qt, :], op=ALU.mult)
                        else:
                            P = Pt[cnt[0] % 3]
                            c.act(P[:, :], p[:, :], AF.Exp, [p], [P])
                        c.mm(pden[0:64, :], self.onesb[:, 0:64], P[:, :], kc == 0, kc == nk - 1, [self.onesb, P], [pden])
                        c.mm(pov[0:64, :], VS[:, kc, :], P[:, :], kc == 0, kc == nk - 1, [VS, P], [pov])
                    finish(r, 1, False)
                    kcs = [kc for kc in range(4 * qt - 4, 4 * qt + 4) if kc >= 0]
                    for ii, kc in enumerate(kcs):
                        p = nst()
                        c.mm(p[:, :], KW[64:128, kc * 128:(kc + 1) * 128], QS[r][64:128, qc], True, True, [KW, (QS[r], "q")], [p])
                        e = ex[cnt[0] % 3]
                        c.act(e[:, :], p[:, :], AF.Exp, [p], [e])
                        P = Pt[cnt[0] % 3]
                        c.call("dve", "tensor_tensor", [e, wm], [P], out=P[:, :], in0=e[:, :], in1=wm[:, kc - (4 * qt - 4), :], op=ALU.mult)
                        c.mm(pden[0:64, :], self.onesb[:, 0:64], P[:, :], ii == 0, ii == len(kcs) - 1, [self.onesb, P], [pden])
                        c.mm(pov[0:64, :], VW[:, kc, :], P[:, :], ii == 0, ii == len(kcs) - 1, [VW, P], [pov])
                    finish(r, 2, False)
                    o_ = oh[r % 2]
                    c.act(o_[:, :], acc[r][:, :], AF.Copy, [acc[r]], [o_])
                    c.ld("sp", oT_d[hd * 64:(hd + 1) * 64, qc], o_[:, :], [o_], [(oT_d, (hd, qt))])
    with c.phase():
        wo = c.sbuf("s_wo", [128, KC, D], BF16)
        self.load_w(wo, lambda kc, cs, cn: wo[:, kc, cs:cs + cn], dr["nsa_w_o"], 0, KC, 0, D)
        ot = [c.sbuf(f"s_ot{i}", [128, KC, T], BF16) for i in range(2)]
        xt = [c.sbuf(f"s_xx{i}", [128, KC, T], F32) for i in range(2)]
        ps = [c.psum(f"s_os{i}", [128, 512]) for i in range(4)]
        for tg in range(NQ):
            o, x = ot[tg % 2], xt[tg % 2]
            cs = slice(tg * T, (tg + 1) * T)
            for k in range(KC):
                c.ld("sp", o[:, k, :], oT_d[k * 128:(k + 1) * 128, cs], [oT_d], [(o, k)])
                c.ld("sp", x[:, k, :], xs[k * 128:(k + 1) * 128, cs], [(xs, (k, tg))], [(x, k)])
            for dc in range(KC):
                p = ps[dc % 4]
                for k in range(KC):
                    c.mm(p[:, :], wo[:, k, dc * 128:(dc + 1) * 128], o[:, k, :], k == 0, k == KC - 1, [wo, (o, k)], [p])
                c.call("dve", "tensor_tensor", [p, (x, dc)], [(x, dc)], out=x[:, dc, :], in0=p[:, :], in1=x[:, dc, :], op=ALU.add)
                c.ld("sp", xs[dc * 128:(dc + 1) * 128, cs], x[:, dc, :], [(x, dc)], [(xs, (dc, tg))])


M.nsa = _nsa


S_FULL = 4096
_WNAMES = ["ffn1_norm", "ffn1_w_in", "ffn1_w_out", "mix_norm", "xattn_norm", "mem_norm", "xattn_w_q", "xattn_w_kv", "xattn_w_o",
           "ffn2_norm", "ffn2_w_in", "ffn2_w_out", "pool_w", "pool_b", "pool_scale", "nsa_w_in", "nsa_cmp_pos", "nsa_cmp_w1",
           "nsa_cmp_w2", "nsa_w_o", "gla_w_in", "gla_w_gate_up", "gla_b_gate", "gla_norm", "gla_w_o", "conv_w_in", "conv_b_in",
           "conv_dw", "conv_b_dw", "conv_ln_g", "conv_ln_b", "conv_w_out", "conv_b_out", "final_norm"]


def _host_consts(S):
    import ml_dtypes
    inv = np.zeros((128, 4, 512), np.float32)
    for gi, wn in enumerate((2, 4, 8, 16)):
        inv[:, gi, :] = 1.0 / np.minimum(np.arange(512) + 1, wn)
    sm = np.ones((128, 512), np.float32)
    sm[:, ::64] = 0
    ii = np.arange(128)
    gm = ((ii[:, None] // 64 == ii[None, :] // 64) & (ii[:, None] <= ii[None, :])).astype(np.float32)
    ident = np.eye(128, dtype=np.float32).astype(ml_dtypes.bfloat16)
    NKC, NCP = S // 128, S // 16
    NC2, NCMP, NSL = NCP // 128, S // 16 - 1, S // 64
    bf = ml_dtypes.bfloat16
    key = np.arange(S)
    E = np.zeros((64, S), np.float32)
    E[key // 64, key] = 1.0
    n = np.arange(NCP).reshape(NC2, 128)
    q = np.arange(S)
    vis = ((16 * n[:, :, None] + 31) <= q[None, None, :]) & (n[:, :, None] < NCMP)
    vis = vis.transpose(1, 0, 2).astype(np.float32)
    p = np.arange(128)[:, None, None]
    dd = np.arange(8)[None, :, None]
    f = np.arange(512)[None, None, :]
    rel = 128 * (dd - 4) + p
    wm = ((rel <= f) & (rel > f - 512)).astype(np.float32)
    t = np.arange(S)
    m = np.arange(64)
    cur = t // 64
    forced = (m[None, :] == 0) | (m[None, :] == cur[:, None]) | (m[None, :] == cur[:, None] - 1)
    valid = (m[None, :] * 64 <= t[:, None]) & (m[None, :] < NSL)
    Fm = np.where(forced, 1e4, 0.0).astype(np.float32)
    Um = np.where(valid, 1e30, -1.0).astype(np.float32)
    Fm = Fm.reshape(NKC, 128, 64).transpose(1, 0, 2)
    Um = Um.reshape(NKC, 128, 64).transpose(1, 0, 2)
    cs_ = np.arange(NCP) * 16
    ss_ = np.arange(64) * 64
    ov = np.clip(np.minimum(cs_[:, None] + 32, ss_[None, :] + 64) - np.maximum(cs_[:, None], ss_[None, :]), 0, None) / 32.0
    ov[NCMP:, :] = 0
    ov[:, NSL:] = 0
    ov = ov.reshape(NC2, 128, 64).transpose(1, 0, 2)
    return dict(pool_inv=inv, scanmask=sm, gmask=gm, ident=ident, nsa_E=E.astype(bf), nsa_vis=np.ascontiguousarray(vis).astype(bf),
                nsa_wm=wm.astype(bf), nsa_F=np.ascontiguousarray(Fm), nsa_U=np.ascontiguousarray(Um),
                nsa_ov=np.ascontiguousarray(ov).astype(bf), identf=np.eye(128, dtype=np.float32))


def build_program(S=S_FULL, with_nsa=True):
    nc = bass.Bass("TRN2", target_bir_lowering=False)
    c = Ctx(nc)
    shapes = dict(
        ffn1_norm=[4, D], ffn1_w_in=[4, D, 2 * FF], ffn1_w_out=[4, FF, D], mix_norm=[4, D], xattn_norm=[4, D], mem_norm=[4, D],
        xattn_w_q=[4, D, D], xattn_w_kv=[4, D, 2 * D], xattn_w_o=[4, D, D], ffn2_norm=[4, D], ffn2_w_in=[4, D, 2 * FF],
        ffn2_w_out=[4, FF, D], pool_w=[1024, 256], pool_b=[D], pool_scale=[D], nsa_w_in=[D, 2608], nsa_cmp_pos=[2, 32, 64],
        nsa_cmp_w1=[2, 2048, 256], nsa_cmp_w2=[2, 256, 64], nsa_w_o=[D, D], gla_w_in=[D, 3088], gla_w_gate_up=[16, 512],
        gla_b_gate=[512], gla_norm=[256], gla_w_o=[D, D], conv_w_in=[D, 2 * D], conv_b_in=[2 * D], conv_dw_l=[128, KC, 31],
        conv_b_dw=[D], conv_ln_g=[D], conv_ln_b=[D], conv_w_out=[D, D], conv_b_out=[D], final_norm=[D])
    dr = {k: c.dram(k, v, F32, kind="ExternalInput") for k, v in shapes.items()}
    xin = c.dram("xT", [D, S], F32, kind="ExternalInput")
    memT = c.dram("memT", [D, 256], F32, kind="ExternalInput")
    inv_d = c.dram("pool_inv", [128, 4, 512], F32, kind="ExternalInput")
    cst = dict(scanmask=c.dram("scanmask", [128, 512], F32, kind="ExternalInput"),
               gmask=c.dram("gmask", [128, 128], F32, kind="ExternalInput"),
               ident=c.dram("ident", [128, 128], BF16, kind="ExternalInput"),
               nsa_E=c.dram("nsa_E", [64, S], BF16, kind="ExternalInput"),
               nsa_vis=c.dram("nsa_vis", [128, S // 2048, S], BF16, kind="ExternalInput"),
               nsa_wm=c.dram("nsa_wm", [128, 8, 512], BF16, kind="ExternalInput"),
               nsa_F=c.dram("nsa_F", [128, S // 128, 64], F32, kind="ExternalInput"),
               nsa_U=c.dram("nsa_U", [128, S // 128, 64], F32, kind="ExternalInput"),
               nsa_ov=c.dram("nsa_ov", [128, S // 2048, 64], BF16, kind="ExternalInput"),
               identf=c.dram("identf", [128, 128], F32, kind="ExternalInput"))
    xs = c.dram("xs", [D, S], F32)
    uT = c.dram("uT", [D, 32 + S], F32)
    out = c.dram("outT", [D, S], F32, kind="ExternalOutput")
    m = M(c, S)
    m.consts()

    class Sub:
        pass

    def lw(name, l):
        T = dr[name]
        v = Tile(c, f"{name}{l}", T.t[l], "dram")
        return v

    cur = xin
    for l in range(4):
        m.ffn(cur, xs, dr["ffn1_norm"].t[l], lw("ffn1_w_in", l), lw("ffn1_w_out", l), l)
        cur = xs
        g_ap = dr["mix_norm"].t[l]
        if l == 0:
            m.pool(xs, g_ap, dr["pool_w"], dr["pool_b"].t, dr["pool_scale"].t, inv_d)
        elif l == 1:
            if with_nsa:
                m.nsa(xs, g_ap, dr, cst)
        elif l == 2:
            m.gla(xs, g_ap, dr["gla_w_in"], dr["gla_w_gate_up"], dr["gla_b_gate"].t, dr["gla_norm"].t, dr["gla_w_o"], cst)
        else:
            m.conv(xs, g_ap, dr["conv_w_in"], dr["conv_b_in"].t, dr["conv_dw_l"], dr["conv_b_dw"].t, dr["conv_ln_g"].t,
                   dr["conv_ln_b"].t, dr["conv_w_out"], dr["conv_b_out"].t, uT)
        m.xattn(xs, memT, dr["xattn_norm"].t[l], dr["mem_norm"].t[l], lw("xattn_w_q", l), lw("xattn_w_kv", l), lw("xattn_w_o", l))
        m.ffn(xs, xs, dr["ffn2_norm"].t[l], lw("ffn2_w_in", l), lw("ffn2_w_out", l), l)
    m.final_norm(xs, out, dr["final_norm"].t)
    c.emit()
    return nc, c


def kernel(**inputs):
    S = S_FULL
    x = np.asarray(inputs["x"], np.float32)
    mem = np.asarray(inputs["mem"], np.float32)
    B = x.shape[0]
    nc, c = build_program(S)
    shared = {}
    for k in _WNAMES:
        a = np.ascontiguousarray(np.asarray(inputs[k], np.float32))
        if k in ("pool_w",):
            a = a.reshape(1024, 256)
        elif k in ("pool_b", "pool_scale", "nsa_w_in", "nsa_cmp_pos", "nsa_cmp_w1", "nsa_cmp_w2", "nsa_w_o", "gla_w_in",
                   "gla_w_gate_up", "gla_b_gate", "gla_norm", "gla_w_o", "conv_w_in", "conv_b_in", "conv_b_dw", "conv_ln_g",
                   "conv_ln_b", "conv_w_out", "conv_b_out"):
            a = a[0]
        if k == "conv_dw":
            shared["conv_dw_l"] = np.ascontiguousarray(a[0].T.reshape(KC, 128, 31).transpose(1, 0, 2))
            continue
        shared[k] = np.ascontiguousarray(a)
    shared.update(_host_consts(S))
    in_maps = []
    for b in range(B):
        mp = dict(shared)
        mp["xT"] = np.ascontiguousarray(x[b].T)
        mp["memT"] = np.ascontiguousarray(mem[b].T)
        in_maps.append(mp)
    res = run_bass_kernel_spmd(nc, in_maps, core_ids=list(range(B)))
    outp = np.stack([np.ascontiguousarray(res.results[b]["outT"].T) for b in range(B)], 0)
    return outp.astype(np.float32)
```

```python
from concourse.bass_utils import run_bass_kernel_spmd

import numpy as np
import concourse.bass as bass
import concourse.mybir as mybir

F32 = mybir.dt.float32
BF16 = mybir.dt.bfloat16
AF = mybir.ActivationFunctionType
ALU = mybir.AluOpType
AX = mybir.AxisListType

ENGS = ("pe", "act", "dve", "pool", "sp")


class _St:
    __slots__ = ("w", "r")

    def __init__(self, w=None, r=None):
        self.w = w
        self.r = dict(r or {})

    def copy(self):
        return _St(self.w, self.r)


class DSem:
    def __init__(self, h):
        self.h, self.cnt = h, 0


class Tile:
    def __init__(self, ctx, name, t, space):
        self.ctx, self.name, self.t, self.space = ctx, name, t, space
        self.whole = _St()
        self.regions = {}
        self.dsem = None
        self.dcnt = 0

    def __getitem__(self, idx):
        return self.t[idx]

    def st(self, key):
        if key is None:
            return None
        if key not in self.regions:
            self.regions[key] = self.whole.copy()
        return self.regions[key]


class Ctx:
    def __init__(self, nc):
        self.nc = nc
        self.E = {"pe": nc.tensor, "act": nc.scalar, "dve": nc.vector, "pool": nc.gpsimd, "sp": nc.sync}
        self.ops = []
        self.ecount = {e: 0 for e in ENGS}
        self.tiles = []
        self.same_engine_sync = {"pe": False, "act": True, "dve": True, "pool": True, "sp": False}
        self.bar = {e: set() for e in ENGS}
        self.last = {}

    def barrier(self):
        deps = {("e", e, i) for e, i in self.last.items()}
        for d in getattr(self, "dall", []):
            if d.cnt:
                deps.add(("d", d, d.cnt))
        for e in ENGS:
            self.bar[e] |= deps

    class _Phase:
        def __init__(self, c):
            self.c = c

        def __enter__(self):
            nc = self.c.nc
            self.sv = (nc.sbuf_base, nc.sbuf_top, nc.psum_base, nc.psum_top)
            self.ntiles = len(self.c.tiles)
            return self

        def __exit__(self, *a):
            nc = self.c.nc
            self.c.barrier()
            for T in self.c.tiles[self.ntiles:]:
                if T.space != "dram" and T.dsem is not None:
                    self.c.dfree.append(T.dsem)
                    T.dsem = None
            del self.c.tiles[self.ntiles:]
            nc.sbuf_base, nc.sbuf_top, nc.psum_base, nc.psum_top = self.sv

    def phase(self):
        return Ctx._Phase(self)

    def _nm(self, name):
        self._uid = getattr(self, "_uid", 0) + 1
        return f"{name}_{self._uid}"

    def get_dsem(self):
        if not hasattr(self, "dfree"):
            self.dfree, self.dall = [], []
        if self.dfree:
            return self.dfree.pop()
        d = DSem(self.nc.alloc_semaphore(self._nm("dsem")))
        self.dall.append(d)
        return d

    def sbuf(self, name, shape, dtype):
        name = self._nm(name)
        t = self.nc.alloc_sbuf_tensor(name, list(shape), dtype)
        T = Tile(self, name, t, "sbuf")
        self.tiles.append(T)
        return T

    def psum(self, name, shape, dtype=F32):
        name = self._nm(name)
        t = self.nc.alloc_psum_tensor(name, list(shape), dtype)
        T = Tile(self, name, t, "psum")
        self.tiles.append(T)
        return T

    def dram(self, name, shape, dtype, kind="Internal"):
        t = self.nc.dram_tensor(name, list(shape), dtype, kind=kind)
        T = Tile(self, name, t.ap(), "dram")
        self.tiles.append(T)
        return T

    def _collect(self, eng, reads, writes):
        deps = set()

        def states(T, key):
            if key is None:
                return [T.whole] + list(T.regions.values())
            return [T.st(key)]

        for (T, key) in reads:
            for s in states(T, key):
                if s.w is not None:
                    deps.add(s.w)
        for (T, key) in writes:
            for s in states(T, key):
                if s.w is not None:
                    deps.add(s.w)
                for d in s.r.values():
                    deps.add(d)
        return deps

    def _update(self, me_r, me_w, rkey, reads, writes):
        for (T, key) in reads:
            if key is None:
                T.whole.r[rkey] = me_r
                for s in T.regions.values():
                    s.r[rkey] = me_r
            else:
                T.st(key).r[rkey] = me_r
        for (T, key) in writes:
            if key is None:
                T.whole = _St(me_w)
                T.regions = {}
            else:
                s = T.st(key)
                s.w = me_w
                s.r = {}

    def op(self, eng, fn, reads=(), writes=()):
        reads = [(r, None) if isinstance(r, Tile) else r for r in reads]
        writes = [(w, None) if isinstance(w, Tile) else w for w in writes]
        writes = writes + [(T, None) for (T, k) in reads if T.space == "psum"]
        reads = [(T, k) for (T, k) in reads if T.space != "psum"]
        writes = [(T, None) if T.space == "psum" else (T, k) for (T, k) in writes]
        deps = self._collect(eng, reads, writes)
        deps = {(d[0], d[1], d[1].cnt) if d[0] == "d" else d for d in deps}
        idx = self.ecount[eng]
        self.ecount[eng] += 1
        me = ("e", eng, idx)
        if self.bar[eng]:
            deps |= self.bar[eng]
            self.bar[eng] = set()
        self.last[eng] = idx
        if not self.same_engine_sync[eng]:
            deps = {d for d in deps if not (d[0] == "e" and d[1] == eng)}
        else:
            deps = {d for d in deps if not (d == me)}
        self.ops.append(dict(eng=eng, fn=fn, deps=deps, idx=idx, kind="c"))
        self._update(me, me, eng, reads, writes)

    def call(self, eng, method, reads, writes, *args, **kw):
        f = getattr(self.E[eng], method)
        self.op(eng, lambda: f(*args, **kw), reads=reads, writes=writes)

    def mm(self, out, lhsT, rhs, start, stop, reads, writes):
        f = self.nc.tensor.matmul
        self.op("pe", lambda: f(out, lhsT=lhsT, rhs=rhs, start=start, stop=stop), reads=reads, writes=writes)

    def act(self, out, in_, func, reads, writes, **kw):
        f = self.nc.scalar.activation
        self.op("act", lambda: f(out=out, in_=in_, func=func, **kw), reads=reads, writes=writes)

    def ld(self, q, out, in_, reads, writes, **kw):
        f = self.E[q].dma_start
        self.dma(q, lambda: f(out=out, in_=in_, **kw), reads=reads, writes=writes)

    def dma(self, q, fn, reads=(), writes=()):
        reads = [(r, None) if isinstance(r, Tile) else r for r in reads]
        writes = [(w, None) if isinstance(w, Tile) else w for w in writes]
        deps = self._collect(q, reads, writes)
        deps = {(d[0], d[1], d[1].cnt) if d[0] == "d" else d for d in deps}
        idx = self.ecount[q]
        self.ecount[q] += 1
        cands = [T for (T, _) in writes if T.space != "dram"] + [T for (T, _) in reads if T.space != "dram"] \
            + [T for (T, _) in writes] + [T for (T, _) in reads]
        own = cands[0]
        if own.dsem is None:
            own.dsem = self.get_dsem()
        own.dsem.cnt += 16
        me = ("d", own.dsem, own.dsem.cnt)
        if self.bar[q]:
            deps |= self.bar[q]
            self.bar[q] = set()
        deps = {d for d in deps if not (d[0] == "e" and d[1] == q)}
        self.ops.append(dict(eng=q, fn=fn, deps=deps, idx=idx, kind="d", dsem=own.dsem))
        self._update(me, me, ("d", id(own.dsem)), reads, writes)

    def emit(self, final_waits=()):
        nc = self.nc
        sig = {e: set() for e in ENGS}
        for o in self.ops:
            for d in o["deps"]:
                if d[0] == "e":
                    sig[d[1]].add(d[2])
        rank = {}
        for e in ENGS:
            for i, idx in enumerate(sorted(sig[e])):
                rank[(e, idx)] = i + 1
        sem = {e: nc.alloc_semaphore("c_" + e) for e in ENGS if sig[e]}
        seen = {e: {} for e in ENGS}
        nwaits = 0
        for o in self.ops:
            e = o["eng"]
            eng = self.E[e]
            need = {}
            for d in o["deps"]:
                if d[0] == "e":
                    s, v = sem[d[1]], rank[(d[1], d[2])]
                    k = ("e", d[1])
                else:
                    s, v = d[1].h, d[2]
                    k = ("d", id(d[1]))
                if v > need.get(k, (None, 0))[1]:
                    need[k] = (s, v)
            for k, (s, v) in need.items():
                if seen[e].get(k, 0) >= v:
                    continue
                eng.wait_ge(s, v)
                seen[e][k] = v
                nwaits += 1
            ins = o["fn"]()
            if o["kind"] == "d":
                ins.then_inc(o["dsem"].h, 16)
            elif (e, o["idx"]) in rank:
                ins.then_inc(sem[e], 1)
        for d in getattr(self, "dall", []):
            if d.cnt:
                nc.sync.wait_ge(d.h, d.cnt)
        self.stats = dict(nops=len(self.ops), nwaits=nwaits, counts=dict(self.ecount))


D = 1024
FF = 2816
KC = 8
EPS = 1e-6
PIECES = [(0, 6), (6, 6), (12, 5), (17, 5)]


class M:
    def __init__(self, c, S):
        self.c, self.nc, self.S = c, c.nc, S
        self._stg_i = 0

    def consts(self):
        c, nc = self.c, self.nc
        self.onesb = c.sbuf("onesb", [128, 128], BF16)
        self.eps = c.sbuf("eps_t", [128, 1], F32)
        self.stg = [c.sbuf(f"stg{i}", [128, 1024], F32) for i in range(4)]
        c.call("dve", "memset", [], [self.onesb], self.onesb[:, :], 1.0)
        c.call("dve", "memset", [], [self.eps], self.eps[:, :], EPS)

    def load_w(self, dst, dst_ap_fn, W, r0, nrows_chunks, c0, ncols, key=None):
        c, nc = self.c, self.nc
        for kc in range(nrows_chunks):
            for cs in range(0, ncols, 1024):
                cn = min(1024, ncols - cs)
                st = self.stg[self._stg_i % 4]
                self._stg_i += 1
                c.ld("sp", st[:, 0:cn], W[r0 + kc * 128: r0 + (kc + 1) * 128, c0 + cs: c0 + cs + cn], [W], [st])
                c.call("pool", "tensor_copy", [st], [(dst, (key, kc, cs))], out=dst_ap_fn(kc, cs, cn), in_=st[:, 0:cn])

    def load_vec(self, name, v_ap, n):
        c = self.c
        t = c.sbuf(name, [128, n], F32)
        c.ld("sp", t[:, :], v_ap.rearrange("(k p) -> p k", p=128), [], [t], allow_slow_non_contiguous=True)
        return t

    def norm_cols(self, xt, xn, g, ncols, xcols, ocols, ps, sqb, rs, nk=KC, dim=D, gk=None, key=None):
        c, nc = self.c, self.nc
        for k in range(nk):
            s = sqb[k % 2]
            c.act(s[:, 0:ncols], xt[:, k, xcols], AF.Square, [xt], [s])
            c.mm(ps[:, 0:ncols], self.onesb[:, :], s[:, 0:ncols], k == 0, k == nk - 1, [self.onesb, s], [ps])
        c.act(rs[:, 0:ncols], ps[:, 0:ncols], AF.Sqrt, [ps, self.eps], [rs], bias=self.eps[:, 0:1], scale=1.0 / dim)
        c.call("dve", "reciprocal", [rs], [rs], out=rs[:, 0:ncols], in_=rs[:, 0:ncols])
        for k in range(nk):
            c.call("dve", "scalar_tensor_tensor", [xt, g, rs], [(xn, key)], out=xn[:, k, ocols], in0=xt[:, k, xcols],
                   scalar=g[:, (k if gk is None else gk(k)):(k if gk is None else gk(k)) + 1], in1=rs[:, 0:ncols],
                   op0=ALU.mult, op1=ALU.mult)

    def ffn(self, src, dst, g_ap, win, wout, li):
        c, nc, S = self.c, self.nc, self.S
        TG = 256
        H = min(2048, S)
        NTG = H // TG
        with c.phase():
            x = c.sbuf("f_x", [128, KC, H], F32)
            xn = c.sbuf("f_xn", [128, KC, H], BF16)
            g = self.load_vec("f_g", g_ap, KC)
            sqb = [c.sbuf(f"f_sq{i}", [128, TG], BF16) for i in range(2)]
            rstd = [c.sbuf(f"f_rs{i}", [128, TG], F32) for i in range(2)]
            wi = [c.sbuf(f"f_wi{i}", [128, KC, 2, 768], BF16) for i in range(2)]
            wo = [c.sbuf(f"f_wo{i}", [128, 6, D], BF16) for i in range(2)]
            sil = [c.sbuf(f"f_sil{i}", [128, TG], F32) for i in range(2)]
            act = [c.sbuf(f"f_act{i}", [128, 6, TG], BF16) for i in range(2)]
            ph = [c.psum(f"f_ph{i}", [128, 512]) for i in range(4)]
            py = [c.psum(f"f_py{i}", [128, 2, TG]) for i in range(4)]
            pcount = 0
            hcount = 0
            for half in range(S // H):
                hs = half * H
                for k in range(KC):
                    c.ld("sp", x[:, k, :], src[k * 128:(k + 1) * 128, hs:hs + H], [(src, (k, half))], [(x, k)])

                def load_piece(pi, b):
                    f0, nf = PIECES[pi]
                    for h in range(2):
                        self.load_w(wi[b], lambda kc, cs, cn, h=h, b=b: wi[b][:, kc, h, cs:cs + cn], win, 0, KC,
                                    h * FF + f0 * 128, nf * 128, key=h)
                    self.load_w(wo[b], lambda kc, cs, cn, b=b: wo[b][:, kc, cs:cs + cn], wout, f0 * 128, nf, 0, D)

                load_piece(0, pcount % 2)
                for tg in range(NTG):
                    ts = slice(tg * TG, (tg + 1) * TG)
                    self.norm_cols(x, xn, g, TG, ts, ts, ph[tg % 4], sqb, rstd[tg % 2], key=tg)
                for pi, (f0, nf) in enumerate(PIECES):
                    b = pcount % 2
                    pcount += 1
                    if pi + 1 < len(PIECES):
                        load_piece(pi + 1, pcount % 2)
                    def h_part(tg, b=b, nf=nf):
                        nonlocal hcount
                        ts = slice(tg * TG, (tg + 1) * TG)
                        a = act[tg % 2]
                        for f in range(nf):
                            pp = (ph[(2 * hcount) % 4], ph[(2 * hcount + 1) % 4])
                            hcount += 1
                            for h in range(2):
                                for k in range(KC):
                                    c.mm(pp[h][:, 0:TG], wi[b][:, k, h, f * 128:(f + 1) * 128], xn[:, k, ts],
                                         k == 0, k == KC - 1, [wi[b], (xn, tg)], [pp[h]])
                            s_ = sil[hcount % 2]
                            c.act(s_[:, :], pp[0][:, 0:TG], AF.Silu, [pp[0]], [s_])
                            c.call("dve", "tensor_tensor", [pp[1], s_], [(a, f)], out=a[:, f, :], in0=pp[1][:, 0:TG],
                                   in1=s_[:, :], op=ALU.mult)

                    def y_part(tg, b=b, nf=nf):
                        ts = slice(tg * TG, (tg + 1) * TG)
                        a = act[tg % 2]
                        for dc in range(KC):
                            for f in range(nf):
                                c.mm(py[dc // 2][:, dc % 2, :], wo[b][:, f, dc * 128:(dc + 1) * 128], a[:, f, :],
                                     f == 0, f == nf - 1, [wo[b], (a, f)], [py[dc // 2]])
                        for k in range(KC):
                            c.call("dve", "scalar_tensor_tensor", [py[k // 2], (x, k)], [(x, k)], out=x[:, k, ts],
                                   in0=py[k // 2][:, k % 2, :], scalar=0.5, in1=x[:, k, ts], op0=ALU.mult, op1=ALU.add)

                    h_part(0)
                    for tg in range(NTG):
                        if tg + 1 < NTG:
                            h_part(tg + 1)
                        y_part(tg)
                for k in range(KC):
                    c.ld("sp", dst[k * 128:(k + 1) * 128, hs:hs + H], x[:, k, :], [(x, k)], [(dst, (k, half))])

    def xattn(self, xs, memT, gx_ap, gm_ap, wq_d, wkv_d, wo_d):
        c, nc, S = self.c, self.nc, self.S
        T = 512
        with c.phase():
            gx = self.load_vec("a_gx", gx_ap, KC)
            gm = self.load_vec("a_gm", gm_ap, KC)
            mem = c.sbuf("a_mem", [128, KC, 256], F32)
            memn = c.sbuf("a_memn", [128, KC, 256], BF16)
            wkv = c.sbuf("a_wkv", [128, KC, 2048], BF16)
            wq = c.sbuf("a_wq", [128, KC, D], BF16)
            wo = c.sbuf("a_wo", [128, KC, D], BF16)
            kT = c.sbuf("a_kT", [128, KC, 256], BF16)
            V = c.sbuf("a_V", [128, 2, D], BF16)
            sqb = [c.sbuf(f"a_sq{i}", [128, T], BF16) for i in range(2)]
            rs = c.sbuf("a_rs", [128, T], F32)
            xt = [c.sbuf(f"a_x{i}", [128, KC, T], F32) for i in range(2)]
            xn = c.sbuf("a_xn", [128, KC, T], BF16)
            qT = c.sbuf("a_qT", [128, KC, T], BF16)
            pT = [c.sbuf(f"a_pT{i}", [128, 2, T], BF16) for i in range(4)]
            rdens = [c.sbuf(f"a_rden{i}", [128, T], F32) for i in range(4)]
            oT = c.sbuf("a_oT", [128, KC, T], BF16)
            ps = [c.psum(f"a_ps{i}", [128, 512]) for i in range(8)]
            pi = [0]

            def nps():
                pi[0] += 1
                return ps[pi[0] % 8]

            for k in range(KC):
                c.ld("sp", mem[:, k, :], memT[k * 128:(k + 1) * 128, :], [memT], [(mem, k)])
            self.load_w(wkv, lambda kc, cs, cn: wkv[:, kc, cs:cs + cn], wkv_d, 0, KC, 0, 2048)
            self.load_w(wq, lambda kc, cs, cn: wq[:, kc, cs:cs + cn], wq_d, 0, KC, 0, D)
            self.load_w(wo, lambda kc, cs, cn: wo[:, kc, cs:cs + cn], wo_d, 0, KC, 0, D)
            self.norm_cols(mem, memn, gm, 256, slice(0, 256), slice(0, 256), nps(), sqb, rs)
            for oc in range(KC):
                p = nps()
                for k in range(KC):
                    c.mm(p[:, 0:256], wkv[:, k, oc * 128:(oc + 1) * 128], memn[:, k, :], k == 0, k == KC - 1, [wkv, memn], [p])
                c.call("dve", "tensor_copy", [p], [(kT, oc)], out=kT[:, oc, :], in_=p[:, 0:256])
            for kc in range(2):
                for nb in range(2):
                    p = nps()
                    for k in range(KC):
                        c.mm(p[:, :], memn[:, k, kc * 128:(kc + 1) * 128], wkv[:, k, D + nb * 512: D + (nb + 1) * 512],
                             k == 0, k == KC - 1, [wkv, memn], [p])
                    c.call("dve", "tensor_copy", [p], [(V, (kc, nb))], out=V[:, kc, nb * 512:(nb + 1) * 512], in_=p[:, :])
            def a_load(tg):
                x = xt[tg % 2]
                for k in range(KC):
                    c.ld("sp", x[:, k, :], xs[k * 128:(k + 1) * 128, tg * T:(tg + 1) * T], [(xs, (k, tg))], [(x, k)])
            a_load(0)
            for tg in range(S // T):
                x = xt[tg % 2]
                cs = slice(tg * T, (tg + 1) * T)
                if tg + 1 < S // T:
                    a_load(tg + 1)
                self.norm_cols(x, xn, gx, T, slice(0, T), slice(0, T), nps(), sqb, rs)
                for oc in range(KC):
                    p = nps()
                    for k in range(KC):
                        c.mm(p[:, :], wq[:, k, oc * 128:(oc + 1) * 128], xn[:, k, :], k == 0, k == KC - 1, [wq, xn], [p])
                    c.act(qT[:, oc, :], p[:, :], AF.Copy, [p], [(qT, oc)], scale=1.0 / 16.0)
                for h in range(4):
                    pt = pT[h]
                    for kc in range(2):
                        p = nps()
                        for dc in range(2):
                            c.mm(p[:, :], kT[:, h * 2 + dc, kc * 128:(kc + 1) * 128], qT[:, h * 2 + dc, :], dc == 0, dc == 1,
                                 [kT, (qT, h * 2 + dc)], [p])
                        c.act(pt[:, kc, :], p[:, :], AF.Exp, [p], [(pt, kc)])
                for h in range(4):
                    pt = pT[h]
                    pd = nps()
                    for kc in range(2):
                        c.mm(pd[:, :], self.onesb[:, :], pt[:, kc, :], kc == 0, kc == 1, [self.onesb, (pt, kc)], [pd])
                    c.act(rdens[h][:, :], pd[:, :], AF.Ln, [pd], [rdens[h]])
                for h in range(4):
                    c.act(rdens[h][:, :], rdens[h][:, :], AF.Exp, [rdens[h]], [rdens[h]], scale=-1.0)
                for h in range(4):
                    pt = pT[h]
                    for mc in range(2):
                        p = nps()
                        for kc in range(2):
                            c.mm(p[:, :], V[:, kc, h * 256 + mc * 128: h * 256 + (mc + 1) * 128], pt[:, kc, :], kc == 0, kc == 1,
                                 [V, (pt, kc)], [p])
                        c.call("dve", "tensor_tensor", [p, rdens[h]], [(oT, h * 2 + mc)], out=oT[:, h * 2 + mc, :], in0=p[:, :],
                               in1=rdens[h][:, :], op=ALU.mult)
                for dc in range(KC):
                    p = nps()
                    for k in range(KC):
                        c.mm(p[:, :], wo[:, k, dc * 128:(dc + 1) * 128], oT[:, k, :], k == 0, k == KC - 1, [wo, (oT, k)], [p])
                    c.call("dve", "tensor_tensor", [p, (x, dc)], [(x, dc)], out=x[:, dc, :], in0=p[:, :], in1=x[:, dc, :], op=ALU.add)
                    c.ld("sp", xs[dc * 128:(dc + 1) * 128, cs], x[:, dc, :], [(x, dc)], [(xs, (dc, tg))])

    def final_norm(self, xs, out, g_ap):
        c, nc, S = self.c, self.nc, self.S
        T = 512
        with c.phase():
            g = self.load_vec("n_g", g_ap, KC)
            sqb = [c.sbuf(f"n_sq{i}", [128, T], BF16) for i in range(2)]
            rs = c.sbuf("n_rs", [128, T], F32)
            xt = [c.sbuf(f"n_x{i}", [128, KC, T], F32) for i in range(2)]
            xo = [c.sbuf(f"n_o{i}", [128, KC, T], F32) for i in range(2)]
            ps = [c.psum(f"n_ps{i}", [128, 512]) for i in range(2)]
            def n_load(tg):
                x = xt[tg % 2]
                for k in range(KC):
                    c.ld("sp", x[:, k, :], xs[k * 128:(k + 1) * 128, tg * T:(tg + 1) * T], [(xs, (k, tg))], [(x, k)])
            n_load(0)
            for tg in range(S // T):
                x, o = xt[tg % 2], xo[tg % 2]
                cs = slice(tg * T, (tg + 1) * T)
                if tg + 1 < S // T:
                    n_load(tg + 1)
                self.norm_cols(x, o, g, T, slice(0, T), slice(0, T), ps[tg % 2], sqb, rs)
                for k in range(KC):
                    c.ld("sp", out[k * 128:(k + 1) * 128, cs], o[:, k, :], [o], [(out, (k, tg))])

    def pool(self, xs, g_ap, pw_d, pb_ap, psc_ap, inv_d):
        c, nc, S = self.c, self.nc, self.S
        T = 512
        HL = 16
        WINS = (2, 4, 8, 16)
        with c.phase():
            g = self.load_vec("p_g", g_ap, KC)
            pb = self.load_vec("p_b", pb_ap, KC)
            psc = self.load_vec("p_sc", psc_ap, KC)
            pw = c.sbuf("p_w", [128, KC, 256], BF16)
            self.load_w(pw, lambda kc, cs, cn: pw[:, kc, cs:cs + cn], pw_d, 0, KC, 0, 256)
            inv0 = c.sbuf("p_inv0", [128, 4, T], F32)
            c.ld("sp", inv0[:, :, :], inv_d[:, :, :], [inv_d], [inv0])
            sqb = [c.sbuf(f"p_sq{i}", [128, T], BF16) for i in range(2)]
            rs = c.sbuf("p_rs", [128, T], F32)
            xt = [c.sbuf(f"p_x{i}", [128, KC, HL + T], F32) for i in range(2)]
            hn = c.sbuf("p_hn", [128, KC, HL + T], F32)
            sa = c.sbuf("p_sa", [128, KC, HL + T], F32)
            sb = c.sbuf("p_sb", [128, KC, HL + T], F32)
            pp = c.sbuf("p_p", [128, KC, T], BF16)
            yt = c.sbuf("p_y", [128, T], F32)
            ps = [c.psum(f"p_ps{i}", [128, 512]) for i in range(4)]
            W = HL + T
            ntile = S // T
            for it, j in enumerate(reversed(range(ntile))):
                x = xt[it % 2]
                if j == 0:
                    c.call("dve", "memset", [], [x], x[:, :, 0:HL], 0.0)
                    for k in range(KC):
                        c.ld("sp", x[:, k, HL:W], xs[k * 128:(k + 1) * 128, 0:T], [(xs, (k, 0))], [x])
                else:
                    for k in range(KC):
                        c.ld("sp", x[:, k, :], xs[k * 128:(k + 1) * 128, j * T - HL:(j + 1) * T],
                             [(xs, (k, j)), (xs, (k, j - 1))], [x])
                self.norm_cols(x, hn, g, HL, slice(0, HL), slice(0, HL), ps[0], sqb, rs)
                self.norm_cols(x, hn, g, T, slice(HL, W), slice(HL, W), ps[1], sqb, rs)
                eng = "pool"
                c.call(eng, "tensor_tensor", [hn], [sa], out=sa[:, :, 1:W], in0=hn[:, :, 1:W], in1=hn[:, :, 0:W - 1], op=ALU.add)
                cur = {2: sa}
                c.call(eng, "tensor_tensor", [sa], [sb], out=sb[:, 2:, 3:W], in0=sa[:, 2:, 3:W], in1=sa[:, 2:, 1:W - 2], op=ALU.add)
                s8 = c.sbuf(f"p_s8_{it}", [128, 4, HL + T], F32) if it == 0 else self._s8
                self._s8 = s8
                c.call(eng, "tensor_tensor", [sb], [s8], out=s8[:, :, 7:W], in0=sb[:, 4:, 7:W], in1=sb[:, 4:, 3:W - 4], op=ALU.add)
                s16 = c.sbuf(f"p_s16_{it}", [128, 2, HL + T], F32) if it == 0 else self._s16
                self._s16 = s16
                c.call(eng, "tensor_tensor", [s8], [s16], out=s16[:, :, 15:W], in0=s8[:, 2:, 15:W], in1=s8[:, 2:, 7:W - 8], op=ALU.add)
                srcs = [(sa, 0), (sb, 2), (s8, 0), (s16, 0)]
                for gi in range(4):
                    st_, off = srcs[gi]
                    for kk in range(2):
                        k = gi * 2 + kk
                        if j == 0:
                            c.call("dve", "tensor_tensor", [st_, inv0], [yt], out=yt[:, :], in0=st_[:, off + kk, HL:W],
                                   in1=inv0[:, gi, :], op=ALU.mult)
                            c.call("dve", "tensor_tensor", [yt, hn], [(pp, k)], out=pp[:, k, :], in0=yt[:, :],
                                   in1=hn[:, k, HL:W], op=ALU.subtract)
                        else:
                            c.call("dve", "scalar_tensor_tensor", [st_, hn], [(pp, k)], out=pp[:, k, :],
                                   in0=st_[:, off + kk, HL:W], scalar=1.0 / WINS[gi], in1=hn[:, k, HL:W],
                                   op0=ALU.mult, op1=ALU.subtract)
                for oc in range(KC):
                    gi = oc // 2
                    p = ps[2 + oc % 2]
                    for kk in range(2):
                        c.mm(p[:, :], pw[:, gi * 2 + kk, (oc % 2) * 128:(oc % 2 + 1) * 128], pp[:, gi * 2 + kk, :], kk == 0, kk == 1,
                             [pw, (pp, gi * 2 + kk)], [p])
                    c.call("dve", "tensor_scalar", [p, pb, psc], [yt], out=yt[:, :], in0=p[:, :], scalar1=pb[:, oc:oc + 1],
                           scalar2=psc[:, oc:oc + 1], op0=ALU.add, op1=ALU.mult)
                    c.call("dve", "tensor_tensor", [yt, x], [x], out=x[:, oc, HL:W], in0=yt[:, :], in1=x[:, oc, HL:W], op=ALU.add)
                    c.ld("sp", xs[oc * 128:(oc + 1) * 128, j * T:(j + 1) * T], x[:, oc, HL:W], [x], [(xs, (oc, j))])

    def conv(self, xs, g_ap, win_d, bin_ap, dw_d, bdw_ap, lng_ap, lnb_ap, wout_d, bout_ap, uT):
        c, nc, S = self.c, self.nc, self.S
        T = 512
        HL = 32
        with c.phase():
            g = self.load_vec("c_g", g_ap, KC)
            b_in = self.load_vec("c_bin", bin_ap, 2 * KC)
            win = c.sbuf("c_win", [128, KC, 2048], BF16)
            self.load_w(win, lambda kc, cs, cn: win[:, kc, cs:cs + cn], win_d, 0, KC, 0, 2048)
            sqb = [c.sbuf(f"c_sq{i}", [128, T], BF16) for i in range(2)]
            rs = c.sbuf("c_rs", [128, T], F32)
            xt = [c.sbuf(f"c_x{i}", [128, KC, T], F32) for i in range(2)]
            xn = c.sbuf("c_xn", [128, KC, T], BF16)
            sg = [c.sbuf(f"c_sg{i}", [128, T], F32) for i in range(2)]
            ut = [c.sbuf(f"c_u{i}", [128, KC, T], F32) for i in range(2)]
            zt = c.sbuf("c_z", [128, HL], F32)
            ps = [c.psum(f"c_ps{i}", [128, 512]) for i in range(8)]
            pi = [0]

            def nps():
                pi[0] += 1
                return ps[pi[0] % 8]
            c.call("dve", "memset", [], [zt], zt[:, :], 0.0)
            for k in range(KC):
                c.ld("sp", uT[k * 128:(k + 1) * 128, 0:HL], zt[:, :], [zt], [(uT, (k, -1))])
            def c_load(tg):
                x = xt[tg % 2]
                for k in range(KC):
                    c.ld("sp", x[:, k, :], xs[k * 128:(k + 1) * 128, tg * T:(tg + 1) * T], [(xs, (k, tg))], [(x, k)])
            c_load(0)
            for tg in range(S // T):
                x, u = xt[tg % 2], ut[tg % 2]
                cs = slice(tg * T, (tg + 1) * T)
                if tg + 1 < S // T:
                    c_load(tg + 1)
                self.norm_cols(x, xn, g, T, slice(0, T), slice(0, T), nps(), sqb, rs)
                for oc in range(KC):
                    pa, pg = nps(), nps()
                    for k in range(KC):
                        c.mm(pa[:, :], win[:, k, oc * 128:(oc + 1) * 128], xn[:, k, :], k == 0, k == KC - 1, [win, xn], [pa])
                    for k in range(KC):
                        c.mm(pg[:, :], win[:, k, D + oc * 128:D + (oc + 1) * 128], xn[:, k, :], k == 0, k == KC - 1, [win, xn], [pg])
                    s = sg[oc % 2]
                    c.act(s[:, :], pg[:, :], AF.Sigmoid, [pg, b_in], [s], bias=b_in[:, KC + oc:KC + oc + 1])
                    c.call("dve", "scalar_tensor_tensor", [pa, b_in, s], [(u, oc)], out=u[:, oc, :], in0=pa[:, :],
                           scalar=b_in[:, oc:oc + 1], in1=s[:, :], op0=ALU.add, op1=ALU.mult)
                    c.ld("sp", uT[oc * 128:(oc + 1) * 128, HL + tg * T: HL + (tg + 1) * T], u[:, oc, :], [(u, oc)], [(uT, (oc, tg))])
        with c.phase():
            dw = c.sbuf("c_dw", [128, KC, 31], F32)
            c.ld("sp", dw[:, :, :], dw_d[:, :, :], [dw_d], [dw])
            bdw = self.load_vec("c_bdw", bdw_ap, KC)
            lng = self.load_vec("c_lng", lng_ap, KC)
            lnb = self.load_vec("c_lnb", lnb_ap, KC)
            bout = self.load_vec("c_bout", bout_ap, KC)
            wout = c.sbuf("c_wout", [128, KC, D], BF16)
            self.load_w(wout, lambda kc, cs, cn: wout[:, kc, cs:cs + cn], wout_d, 0, KC, 0, D)
            uh = [c.sbuf(f"c_uh{i}", [128, KC, HL + T], F32) for i in range(2)]
            v = c.sbuf("c_v", [128, KC, T], F32)
            vb = [c.sbuf(f"c_vb{i}", [128, T], BF16) for i in range(2)]
            vq = [c.sbuf(f"c_vq{i}", [128, T], BF16) for i in range(2)]
            mean = c.sbuf("c_mean", [128, T], F32)
            var = c.sbuf("c_var", [128, T], F32)
            t1 = [c.sbuf(f"c_t1{i}", [128, T], F32) for i in range(2)]
            sv = c.sbuf("c_sv", [128, KC, T], BF16)
            xt = [c.sbuf(f"c_xx{i}", [128, KC, T], F32) for i in range(2)]
            yt = c.sbuf("c_yt", [128, T], F32)
            ps = [c.psum(f"c_qs{i}", [128, 512]) for i in range(6)]
            def b_load(tg):
                uu, x = uh[tg % 2], xt[tg % 2]
                for k in range(KC):
                    c.ld("sp", uu[:, k, :], uT[k * 128:(k + 1) * 128, tg * T: tg * T + HL + T],
                         [(uT, (k, tg)), (uT, (k, tg - 1))], [(uu, k)])
                    c.ld("sp", x[:, k, :], xs[k * 128:(k + 1) * 128, tg * T:(tg + 1) * T], [(xs, (k, tg))], [(x, k)])
            b_load(0)
            for tg in range(S // T):
                uu, x = uh[tg % 2], xt[tg % 2]
                cs = slice(tg * T, (tg + 1) * T)
                if tg + 1 < S // T:
                    b_load(tg + 1)
                for k0 in range(0, KC, 2):
                    for j in range(31):
                        for k in (k0, k0 + 1):
                            if j == 0:
                                c.call("dve", "tensor_scalar", [(uu, k), dw, bdw], [(v, k)], out=v[:, k, :], in0=uu[:, k, 2:2 + T],
                                       scalar1=dw[:, k, 0:1], scalar2=bdw[:, k:k + 1], op0=ALU.mult, op1=ALU.add)
                            else:
                                c.call("dve", "scalar_tensor_tensor", [(uu, k), dw, (v, k)], [(v, k)], out=v[:, k, :],
                                       in0=uu[:, k, 2 + j:2 + j + T], scalar=dw[:, k, j:j + 1], in1=v[:, k, :], op0=ALU.mult, op1=ALU.add)
                pm, pq = ps[0], ps[1]
                for k in range(KC):
                    c.act(vb[k % 2][:, :], v[:, k, :], AF.Copy, [(v, k)], [vb[k % 2]])
                    c.act(vq[k % 2][:, :], v[:, k, :], AF.Square, [(v, k)], [vq[k % 2]])
                    c.mm(pm[:, :], self.onesb[:, :], vb[k % 2][:, :], k == 0, k == KC - 1, [self.onesb, vb[k % 2]], [pm])
                    c.mm(pq[:, :], self.onesb[:, :], vq[k % 2][:, :], k == 0, k == KC - 1, [self.onesb, vq[k % 2]], [pq])
                c.act(mean[:, :], pm[:, :], AF.Copy, [pm], [mean], scale=1.0 / D)
                c.call("dve", "tensor_tensor", [mean], [var], out=var[:, :], in0=mean[:, :], in1=mean[:, :], op=ALU.mult)
                c.call("dve", "scalar_tensor_tensor", [pq, var], [var], out=var[:, :], in0=pq[:, :], scalar=1.0 / D, in1=var[:, :],
                       op0=ALU.mult, op1=ALU.subtract)
                c.act(var[:, :], var[:, :], AF.Sqrt, [var, self.eps], [var], bias=self.eps[:, 0:1], scale=1.0)
                c.call("dve", "reciprocal", [var], [var], out=var[:, :], in_=var[:, :])
                for k in range(KC):
                    t = t1[k % 2]
                    c.call("dve", "tensor_tensor", [(v, k), mean], [t], out=t[:, :], in0=v[:, k, :], in1=mean[:, :], op=ALU.subtract)
                    c.call("dve", "tensor_tensor", [t, var], [t], out=t[:, :], in0=t[:, :], in1=var[:, :], op=ALU.mult)
                    c.act(sv[:, k, :], t[:, :], AF.Silu, [t, lng, lnb], [(sv, k)], scale=lng[:, k:k + 1], bias=lnb[:, k:k + 1])
                for dc in range(KC):
                    p = ps[2 + dc % 4]
                    for k in range(KC):
                        c.mm(p[:, :], wout[:, k, dc * 128:(dc + 1) * 128], sv[:, k, :], k == 0, k == KC - 1, [wout, (sv, k)], [p])
                    c.call("dve", "scalar_tensor_tensor", [p, bout, (x, dc)], [(x, dc)], out=x[:, dc, :], in0=p[:, :],
                           scalar=bout[:, dc:dc + 1], in1=x[:, dc, :], op0=ALU.add, op1=ALU.add)
                    c.ld("sp", xs[dc * 128:(dc + 1) * 128, cs], x[:, dc, :], [(x, dc)], [(xs, (dc, tg))])


def _gla(self, xs, g_ap, win_d, wgu_d, bg_ap, ng_ap, wo_d, cst):
    c, nc, S = self.c, self.nc, self.S
    T = 512
    with c.phase():
        g = self.load_vec("g_g", g_ap, KC)
        bg = self.load_vec("g_bg", bg_ap, 4)
        ng = self.load_vec("g_ng", ng_ap, 2)
        nbg = c.sbuf("g_nbg", [128, 4], F32)
        c.call("dve", "tensor_scalar", [bg], [nbg], out=nbg[:, :], in0=bg[:, :], scalar1=-1.0, scalar2=None, op0=ALU.mult)
        win = c.sbuf("g_win", [128, KC, 3088], BF16)
        self.load_w(win, lambda kc, cs, cn: win[:, kc, cs:cs + cn], win_d, 0, KC, 0, 3088)
        wo = c.sbuf("g_wo", [128, KC, D], BF16)
        self.load_w(wo, lambda kc, cs, cn: wo[:, kc, cs:cs + cn], wo_d, 0, KC, 0, D)
        wgu = c.sbuf("g_wgu", [16, 512], BF16)
        st = self.stg[0]
        c.ld("sp", st[0:16, 0:512], wgu_d[:, :], [wgu_d], [st])
        c.call("pool", "tensor_copy", [st], [wgu], out=wgu[:, :], in_=st[0:16, 0:512])
        smask = c.sbuf("g_smask", [128, T], F32)
        c.ld("sp", smask[:, :], cst["scanmask"][:, :], [], [smask])
        gmask = c.sbuf("g_gmask", [128, 128], F32)
        c.ld("sp", gmask[:, :], cst["gmask"][:, :], [], [gmask])
        ident = c.sbuf("g_ident", [128, 128], BF16)
        c.ld("sp", ident[:, :], cst["ident"][:, :], [], [ident])
        sqb = [c.sbuf(f"g_sq{i}", [128, T], BF16) for i in range(2)]
        rs = c.sbuf("g_rs", [128, T], F32)
        xt = [c.sbuf(f"g_x{i}", [128, KC, T], F32) for i in range(1)]
        xn = c.sbuf("g_xn", [128, KC, T], BF16)
        gdT = c.sbuf("g_gd", [16, T], BF16)
        bT = c.sbuf("g_bT", [128, 4, T], F32)
        e1 = [c.sbuf(f"g_e{i}", [128, T], F32) for i in range(2)]
        nbl = c.sbuf("g_nbl", [128, 4, 8], F32)
        dec = c.sbuf("g_dec", [128, 4, 8], F32)
        qt_ = c.sbuf("g_qt", [128, 4, T], BF16)
        kt_ = c.sbuf("g_kt", [128, 4, T], BF16)
        kdT = c.sbuf("g_kdT", [128, 4, T], BF16)
        kd = c.sbuf("g_kd", [128, 4, 4, 128], BF16)
        vtok = c.sbuf("g_vtok", [128, 4, D], BF16)
        sog = c.sbuf("g_sog", [128, KC, T], BF16)
        o = c.sbuf("g_o", [128, KC, T], F32)
        on = o
        ob = c.sbuf("g_ob", [128, KC, T], BF16)
        At = [c.sbuf(f"g_At{i}", [128, 128], BF16) for i in range(2)]
        Sf = c.sbuf("g_S", [128, 4, 256], F32)
        Sb = [c.sbuf(f"g_Sb{i}", [128, 4, 256], BF16) for i in range(2)]
        c.call("dve", "memset", [], [Sf], Sf[:, :, :], 0.0)
        c.call("dve", "memset", [], [Sb[0]], Sb[0][:, :, :], 0.0)
        ps = [c.psum(f"g_ps{i}", [128, 512]) for i in range(7)]
        ptr = c.psum("g_ptr", [128, 4, 128], BF16)
        pi = [0]

        def nps():
            pi[0] += 1
            return ps[pi[0] % 7]
        nchunk = 0
        for tg in range(S // T):
            x = xt[0]
            cs = slice(tg * T, (tg + 1) * T)
            for k in range(KC):
                c.ld("sp", x[:, k, :], xs[k * 128:(k + 1) * 128, cs], [(xs, (k, tg))], [(x, k)])
            self.norm_cols(x, xn, g, T, slice(0, T), slice(0, T), nps(), sqb, rs)
            p = nps()
            for k in range(KC):
                c.mm(p[0:16, :], win[:, k, 3072:3088], xn[:, k, :], k == 0, k == KC - 1, [win, xn], [p])
            c.call("dve", "tensor_copy", [p], [gdT], out=gdT[:, :], in_=p[0:16, :])
            for h in range(4):
                p = nps()
                c.mm(p[:, :], wgu[:, h * 128:(h + 1) * 128], gdT[:, :], True, True, [wgu, gdT], [p])
                e = e1[h % 2]
                c.act(e[:, :], p[:, :], AF.Exp, [p, nbg], [e], scale=-1.0, bias=nbg[:, h:h + 1])
                c.act(e[:, :], e[:, :], AF.Ln, [e], [e], bias=1.0, scale=1.0)
                c.call("dve", "tensor_tensor_scan", [smask, e], [(bT, h)], out=bT[:, h, :], data0=smask[:, :], data1=e[:, :],
                       initial=0.0, op0=ALU.mult, op1=ALU.add)
                c.call("dve", "tensor_scalar", [(bT, h)], [(nbl, h)], out=nbl[:, h, :], in0=bT[:, h, 63::64], scalar1=-1.0 / 16,
                       scalar2=None, op0=ALU.mult)
                c.act(dec[:, h, :], nbl[:, h, :], AF.Exp, [(nbl, h)], [(dec, h)])
                p = nps()
                for k in range(KC):
                    c.mm(p[:, :], win[:, k, h * 128:(h + 1) * 128], xn[:, k, :], k == 0, k == KC - 1, [win, xn], [p])
                e = e1[(h + 1) % 2]
                c.act(e[:, :], bT[:, h, :], AF.Exp, [(bT, h)], [e], scale=-1.0 / 16)
                c.call("dve", "scalar_tensor_tensor", [p, e], [(qt_, h)], out=qt_[:, h, :], in0=p[:, :], scalar=128 ** -0.5,
                       in1=e[:, :], op0=ALU.mult, op1=ALU.mult)
                p = nps()
                for k in range(KC):
                    c.mm(p[:, :], win[:, k, 512 + h * 128:512 + (h + 1) * 128], xn[:, k, :], k == 0, k == KC - 1, [win, xn], [p])
                e = e1[h % 2]
                c.act(e[:, :], bT[:, h, :], AF.Exp, [(bT, h)], [e], scale=1.0 / 16)
                c.call("dve", "tensor_tensor", [p, e], [(kt_, h)], out=kt_[:, h, :], in0=p[:, :], in1=e[:, :], op=ALU.mult)
                for n in range(8):
                    c.act(e[:, n * 64:(n + 1) * 64], bT[:, h, n * 64:(n + 1) * 64], AF.Exp, [(bT, h), (nbl, h)], [e],
                          scale=1.0 / 16, bias=nbl[:, h, n:n + 1])
                c.call("dve", "tensor_tensor", [p, e], [(kdT, h)], out=kdT[:, h, :], in0=p[:, :], in1=e[:, :], op=ALU.mult)
                for tt in range(4):
                    c.op("pe", (lambda o_=ptr[:, tt, :], i_=kdT[:, h, tt * 128:(tt + 1) * 128], id_=ident[:, :]:
                                nc.tensor.transpose(o_, i_, id_)), reads=[(kdT, h), ident], writes=[ptr])
                c.call("dve", "tensor_copy", [ptr], [(kd, h)], out=kd[:, h, :, :], in_=ptr[:, :, :])
            for tt in range(4):
                for nb in range(2):
                    p = nps()
                    for k in range(KC):
                        c.mm(p[:, :], xn[:, k, tt * 128:(tt + 1) * 128], win[:, k, 1024 + nb * 512:1024 + (nb + 1) * 512],
                             k == 0, k == KC - 1, [win, xn], [p])
                    c.call("dve", "tensor_copy", [p], [(vtok, (tt, nb))], out=vtok[:, tt, nb * 512:(nb + 1) * 512], in_=p[:, :])
            for oc in range(KC):
                p = nps()
                for k in range(KC):
                    c.mm(p[:, :], win[:, k, 2048 + oc * 128:2048 + (oc + 1) * 128], xn[:, k, :], k == 0, k == KC - 1, [win, xn], [p])
                c.act(sog[:, oc, :], p[:, :], AF.Silu, [p], [(sog, oc)])
            for tt in range(4):
                for h in range(4):
                    pc = slice(tt * 128, (tt + 1) * 128)
                    pa = nps()
                    c.mm(pa[:, 0:128], kt_[:, h, pc], qt_[:, h, pc], True, True, [(kt_, h), (qt_, h)], [pa])
                    A = At[(tt * 4 + h) % 2]
                    c.call("dve", "tensor_tensor", [pa, gmask], [A], out=A[:, :], in0=pa[:, 0:128], in1=gmask[:, :], op=ALU.mult)
                    n0 = nchunk + 2 * tt
                    po = [nps(), nps()]
                    for mc in range(2):
                        c.mm(po[mc][:, 0:128], vtok[:, tt, h * 256 + mc * 128:h * 256 + (mc + 1) * 128], A[:, :], True, False,
                             [vtok, A], [po[mc]])
                        c.mm(po[mc][:, 0:64], Sb[0][:, h, mc * 128:(mc + 1) * 128], qt_[:, h, tt * 128:tt * 128 + 64], False, False,
                             [(Sb[0], h), (qt_, h)], [po[mc]])
                    pk = nps()
                    c.mm(pk[:, 0:256], kd[0:64, h, tt, :], vtok[0:64, tt, h * 256:(h + 1) * 256], True, True, [(kd, h), vtok], [pk])
                    c.call("dve", "scalar_tensor_tensor", [pk, (Sf, h), (dec, h)], [(Sf, h)], out=Sf[:, h, :], in0=Sf[:, h, :],
                           scalar=dec[:, h, 2 * tt:2 * tt + 1], in1=pk[:, 0:256], op0=ALU.mult, op1=ALU.add)
                    c.act(Sb[1][:, h, :], Sf[:, h, :], AF.Copy, [(Sf, h)], [(Sb[1], h)])
                    for mc in range(2):
                        c.mm(po[mc][:, 64:128], Sb[1][:, h, mc * 128:(mc + 1) * 128], qt_[:, h, tt * 128 + 64:(tt + 1) * 128], False, True,
                             [(Sb[1], h), (qt_, h)], [po[mc]])
                        c.act(o[:, h * 2 + mc, pc], po[mc][:, 0:128], AF.Copy, [po[mc]], [(o, (h, mc, tt))])
                    pk = nps()
                    c.mm(pk[:, 0:256], kd[64:128, h, tt, :], vtok[64:128, tt, h * 256:(h + 1) * 256], True, True, [(kd, h), vtok], [pk])
                    c.call("dve", "scalar_tensor_tensor", [pk, (Sf, h), (dec, h)], [(Sf, h)], out=Sf[:, h, :], in0=Sf[:, h, :],
                           scalar=dec[:, h, 2 * tt + 1:2 * tt + 2], in1=pk[:, 0:256], op0=ALU.mult, op1=ALU.add)
                    c.act(Sb[0][:, h, :], Sf[:, h, :], AF.Copy, [(Sf, h)], [(Sb[0], h)])
            nchunk += 8
            for h in range(4):
                class _V:
                    pass
                for kk in range(2):
                    s = sqb[kk % 2]
                    c.act(s[:, :], o[:, 2 * h + kk, :], AF.Square, [o], [s])
                    pn = ps[0] if kk == 0 else pn
                    c.mm(pn[:, :], self.onesb[:, :], s[:, :], kk == 0, kk == 1, [self.onesb, s], [pn])
                c.act(rs[:, :], pn[:, :], AF.Sqrt, [pn, self.eps], [rs], bias=self.eps[:, 0:1], scale=1.0 / 256)
                c.call("dve", "reciprocal", [rs], [rs], out=rs[:, :], in_=rs[:, :])
                for kk in range(2):
                    k = 2 * h + kk
                    c.call("dve", "scalar_tensor_tensor", [o, ng, rs], [(on, k)], out=on[:, k, :], in0=o[:, k, :], scalar=ng[:, kk:kk + 1],
                           in1=rs[:, :], op0=ALU.mult, op1=ALU.mult)
                    c.call("dve", "tensor_tensor", [(on, k), (sog, k)], [(ob, k)], out=ob[:, k, :], in0=on[:, k, :], in1=sog[:, k, :], op=ALU.mult)
            for dc in range(KC):
                p = nps()
                for k in range(KC):
                    c.mm(p[:, :], wo[:, k, dc * 128:(dc + 1) * 128], ob[:, k, :], k == 0, k == KC - 1, [wo, (ob, k)], [p])
                c.call("dve", "tensor_tensor", [p, (x, dc)], [(x, dc)], out=x[:, dc, :], in0=p[:, :], in1=x[:, dc, :], op=ALU.add)
                c.ld("sp", xs[dc * 128:(dc + 1) * 128, cs], x[:, dc, :], [(x, dc)], [(xs, (dc, tg))])


M.gla = _gla


def _nsa(self, xs, g_ap, dr, cst):
    c, nc, S = self.c, self.nc, self.S
    T = 512
    NQ, NKC, NCMP, NCP = S // T, S // 128, S // 16 - 1, S // 16
    NC2 = NCP // 128
    if not hasattr(self, "_nsa_d"):
        self._nsa_d = dict(qT=c.dram("n_qT", [1024, S], BF16), kvT=c.dram("n_kvT", [1536, S], BF16),
                           vtok=c.dram("n_vtok", [S, 512], BF16), gT=c.dram("n_gT", [48, S], F32),
                           oT=c.dram("n_oT", [1024, S], BF16))
    qT_d, kvT_d, vtok_d, gT_d, oT_d = (self._nsa_d[k] for k in ("qT", "kvT", "vtok", "gT", "oT"))
    with c.phase():
        g = self.load_vec("s_g", g_ap, KC)
        win = c.sbuf("s_win", [128, KC, 2608], BF16)
        self.load_w(win, lambda kc, cs, cn: win[:, kc, cs:cs + cn], dr["nsa_w_in"], 0, KC, 0, 2608)
        sqb = [c.sbuf(f"s_sq{i}", [128, T], BF16) for i in range(2)]
        rs = c.sbuf("s_rs", [128, T], F32)
        xt = [c.sbuf(f"s_x{i}", [128, KC, T], F32) for i in range(2)]
        xn = c.sbuf("s_xn", [128, KC, T], BF16)
        qo = [c.sbuf(f"s_qo{i}", [128, T], BF16) for i in range(3)]
        go = c.sbuf("s_go", [48, T], F32)
        ps = [c.psum(f"s_ps{i}", [128, 512]) for i in range(8)]
        pi = [0]

        def nps():
            pi[0] += 1
            return ps[pi[0] % 8]
        qi = 0

        def s_load(tg):
            x = xt[tg % 2]
            for k in range(KC):
                c.ld("sp", x[:, k, :], xs[k * 128:(k + 1) * 128, tg * T:(tg + 1) * T], [(xs, (k, tg))], [(x, k)])
        s_load(0)
        for tg in range(NQ):
            x = xt[tg % 2]
            cs = slice(tg * T, (tg + 1) * T)
            if tg + 1 < NQ:
                s_load(tg + 1)
            self.norm_cols(x, xn, g, T, slice(0, T), slice(0, T), nps(), sqb, rs)
            for oc in range(20):
                p = nps()
                for k in range(KC):
                    c.mm(p[:, :], win[:, k, oc * 128:(oc + 1) * 128], xn[:, k, :], k == 0, k == KC - 1, [win, xn], [p])
                q = qo[qi % 3]
                qi += 1
                c.act(q[:, :], p[:, :], AF.Copy, [p], [q], scale=(0.125 if oc < 8 else 1.0))
                if oc < 8:
                    c.ld("sp", qT_d[oc * 128:(oc + 1) * 128, cs], q[:, :], [q], [(qT_d, (oc, tg))])
                else:
                    c.ld("sp", kvT_d[(oc - 8) * 128:(oc - 7) * 128, cs], q[:, :], [q], [(kvT_d, (oc, tg))])
            p = nps()
            for k in range(KC):
                c.mm(p[0:48, :], win[:, k, 2560:2608], xn[:, k, :], k == 0, k == KC - 1, [win, xn], [p])
            c.act(go[:, :], p[0:48, :], AF.Sigmoid, [p], [go])
            c.ld("sp", gT_d[:, cs], go[:, :], [go], [(gT_d, tg)])
            for tt in range(4):
                p = nps()
                for half, c0 in enumerate((1792, 2304)):
                    for k in range(KC):
                        c.mm(p[:, half * 256:(half + 1) * 256], xn[:, k, tt * 128:(tt + 1) * 128], win[:, k, c0:c0 + 256],
                             k == 0, k == KC - 1, [win, xn], [p])
                q = qo[qi % 3]
                qi += 1
                c.call("dve", "tensor_copy", [p], [q], out=q[:, :], in_=p[:, :])
                c.ld("sp", vtok_d[tg * T + tt * 128: tg * T + (tt + 1) * 128, :], q[:, :], [q], [(vtok_d, (tg, tt))])
    with c.phase():
        stg0 = self.stg[0]
        kcmpT = c.sbuf("s_kcmpT", [128, 4, NCP], BF16)
        vcmp = c.sbuf("s_vcmp", [128, 4, NC2, 64], BF16)
        with c.phase():
            w1b = c.sbuf("s_w1b", [64, 32, 256], BF16)
            posf = c.sbuf("s_posf", [64, 32], F32)
            posb = c.sbuf("s_posb", [64, 32], BF16)
            c1 = c.sbuf("s_c1", [128, 2], F32)
            w2f = c.sbuf("s_w2f", [128, 2, 64], F32)
            w2b = c.sbuf("s_w2b", [128, 2, 128], BF16)
            hb = c.sbuf("s_hb", [128, 2, NCP], BF16)
            aT = [c.sbuf(f"s_aT{i}", [64, S], BF16) for i in range(2)]
            xg = c.sbuf("s_xg", [128, NCP], F32)
            t1 = c.sbuf("s_t1", [128, NCP], F32)
            t2 = c.sbuf("s_t2", [128, NCP], F32)
            ps = [c.psum(f"s_cs{i}", [128, 512]) for i in range(4)]
            pi = [0]

            def nps():
                pi[0] += 1
                return ps[pi[0] % 4]
            c.call("dve", "memset", [], [hb], hb[:, :, :], 0.0)
            c.call("dve", "memset", [], [w2b], w2b[:, :, :], 0.0)
            for typ in range(2):
                for j in range(32):
                    st = self.stg[self._stg_i % 4]
                    self._stg_i += 1
                    c.ld("sp", st[0:64, 0:256], dr["nsa_cmp_w1"].t[typ, j * 64:(j + 1) * 64, :], [], [st])
                    c.call("pool", "tensor_copy", [st], [(w1b, j)], out=w1b[:, j, :], in_=st[0:64, 0:256])
                c.ld("sp", posf[:, :], dr["nsa_cmp_pos"].t[typ].rearrange("j d -> d j"), [], [posf], allow_slow_non_contiguous=True)
                c.call("dve", "tensor_copy", [posf], [posb], out=posb[:, :], in_=posf[:, :])
                c.ld("sp", w2f[:, :, :], dr["nsa_cmp_w2"].t[typ].rearrange("(c p) d -> p c d", p=128), [], [w2f])
                c.call("dve", "tensor_copy", [w2f], [w2b], out=w2b[:, :, 64:128], in_=w2f[:, :, :])
                for hc in range(2):
                    p = nps()
                    for j in range(32):
                        c.mm(p[:, 0:1], w1b[:, j, hc * 128:(hc + 1) * 128], posb[:, j:j + 1], j == 0, j == 31, [w1b, posb], [p])
                    c.call("dve", "tensor_copy", [p], [(c1, hc)], out=c1[:, hc:hc + 1], in_=p[:, 0:1])
                for gq in range(4):
                    a = aT[gq % 2]
                    c.ld("sp", a[:, :], kvT_d[typ * 256 + gq * 64: typ * 256 + (gq + 1) * 64, :], [kvT_d], [a])
                    for hc in range(2):
                        for n0 in range(0, NCMP, 256):
                            nn = min(256, NCMP - n0)
                            p = nps()
                            for j in range(32):
                                c.mm(p[:, 0:nn], w1b[:, j, hc * 128:(hc + 1) * 128],
                                     a[:, 16 * n0 + j: 16 * n0 + j + 16 * (nn - 1) + 1: 16], j == 0, j == 31, [w1b, a], [p])
                            ns = slice(n0, n0 + nn)
                            c.call("dve", "tensor_scalar", [p, c1], [xg], out=xg[:, ns], in0=p[:, 0:nn], scalar1=c1[:, hc:hc + 1],
                                   scalar2=None, op0=ALU.add)
                            c.act(t1[:, ns], xg[:, ns], AF.Square, [xg], [t1])
                            c.call("dve", "tensor_scalar", [t1], [t1], out=t1[:, ns], in0=t1[:, ns], scalar1=0.044715, scalar2=1.0,
                                   op0=ALU.mult, op1=ALU.add)
                            c.call("dve", "tensor_tensor", [t1, xg], [t2], out=t2[:, ns], in0=t1[:, ns], in1=xg[:, ns], op=ALU.mult)
                            c.act(t2[:, ns], t2[:, ns], AF.Sigmoid, [t2], [t2], scale=1.5957691216)
                            c.call("dve", "tensor_tensor", [t2, xg], [hb], out=hb[:, hc, ns], in0=t2[:, ns], in1=xg[:, ns], op=ALU.mult)
                    if typ == 0:
                        for n0 in range(0, NCMP, 512):
                            nn = min(512, NCMP - n0)
                            p = nps()
                            for hc in range(2):
                                c.mm(p[:, 0:nn], w2b[:, hc, :], hb[:, hc, n0:n0 + nn], hc == 0, hc == 1, [w2b, hb], [p])
                            c.call("dve", "tensor_copy", [p], [(kcmpT, gq)], out=kcmpT[64:128, gq, n0:n0 + nn], in_=p[64:128, 0:nn])
                        c.call("dve", "memset", [], [(kcmpT, gq)], kcmpT[64:128, gq, NCMP:NCP], 0.0)
                    else:
                        for ncx in range(NC2):
                            p = nps()
                            for hc in range(2):
                                c.mm(p[:, 0:64], hb[:, hc, ncx * 128:(ncx + 1) * 128], w2b[:, hc, 64:128], hc == 0, hc == 1, [w2b, hb], [p])
                            c.call("dve", "tensor_copy", [p], [(vcmp, gq)], out=vcmp[:, gq, ncx, :], in_=p[:, 0:64])
        KS = c.sbuf("s_KS", [128, S], BF16)
        KW = c.sbuf("s_KW", [128, S], BF16)
        VS = c.sbuf("s_VS", [128, NKC, 64], BF16)
        VW = c.sbuf("s_VW", [128, NKC, 64], BF16)
        QS = [c.sbuf(f"s_QS{r}", [128, S], BF16) for r in range(4)]
        vis = c.sbuf("s_vis", [128, NC2, S], BF16)
        wm = c.sbuf("s_wm", [128, 8, T], BF16)
        Fc = c.sbuf("s_F", [128, NKC, 64], F32)
        Uc = c.sbuf("s_U", [128, NKC, 64], F32)
        ov = c.sbuf("s_ov", [128, NC2, 64], BF16)
        identf = c.sbuf("s_idf", [128, 128], F32)
        c.ld("sp", vis[:, :, :], cst["nsa_vis"][:, :, :], [], [vis])
        c.ld("sp", wm[:, :, :], cst["nsa_wm"][:, :, :], [], [wm])
        c.ld("sp", Fc[:, :, :], cst["nsa_F"][:, :, :], [], [Fc])
        c.ld("sp", Uc[:, :, :], cst["nsa_U"][:, :, :], [], [Uc])
        c.ld("sp", ov[:, :, :], cst["nsa_ov"][:, :, :], [], [ov])
        c.ld("sp", identf[:, :], cst["identf"][:, :], [], [identf])
        c.ld("sp", KS[0:64, :], cst["nsa_E"][:, :], [], [(KS, "e")])
        Pc = c.sbuf("s_Pc", [128, 4, NC2, T], BF16)
        Pn = c.sbuf("s_Pn", [128, 4, NC2, T], BF16)
        ex = [c.sbuf(f"s_ex{i}", [128, T], BF16) for i in range(3)]
        Pt = [c.sbuf(f"s_P{i}", [128, T], BF16) for i in range(3)]
        rden = [c.sbuf(f"s_rden{i}", [128, T], F32) for i in range(2)]
        GB = [c.sbuf(f"s_GB{r}", [64, 3, T], F32) for r in range(4)]
        acc = [c.sbuf(f"s_acc{r}", [64, T], F32) for r in range(4)]
        tmp = [c.sbuf(f"s_tmp{i}", [64, T], F32) for i in range(2)]
        oh = [c.sbuf(f"s_oh{i}", [64, T], BF16) for i in range(2)]
        imp2 = c.sbuf("s_imp2", [128, 64], F32)
        impt = c.sbuf("s_impt", [128, 64], F32)
        m8 = c.sbuf("s_m8", [128, 16], F32)
        thr = c.sbuf("s_thr", [128, 1], F32)
        negm = c.sbuf("s_negm", [128, 64], F32)
        tiny = c.sbuf("s_tiny", [128, 1], F32)
        c.call("dve", "memset", [], [tiny], tiny[:, :], 1e-30)
        pst = [c.psum(f"s_st{i}", [128, 512]) for i in range(3)]
        pdens = [c.psum(f"s_pden{i}", [128, 512]) for i in range(2)]
        povs = [c.psum(f"s_pov{i}", [128, 512]) for i in range(2)]
        pim = c.psum("s_pim", [128, 512])
        ptp = pim
        cnt = [0]
        bsel = [0]

        def nst():
            cnt[0] += 1
            return pst[cnt[0] % 3]

        def finish(r, br, first):
            pden, pov = pdens[bsel[0] % 2], povs[bsel[0] % 2]
            bsel[0] += 1
            rd = rden[cnt[0] % 2]
            c.act(rd[0:64, :], pden[0:64, :], AF.Ln, [pden, tiny], [rd], bias=tiny[0:64, 0:1], scale=1.0)
            c.act(rd[0:64, :], rd[0:64, :], AF.Exp, [rd], [rd], scale=-1.0)
            t = tmp[cnt[0] % 2]
            c.call("dve", "tensor_tensor", [pov, rd], [t], out=t[:, :], in0=pov[0:64, :], in1=rd[0:64, :], op=ALU.mult)
            if first:
                c.call("dve", "tensor_tensor", [t, (GB[r], br)], [acc[r]], out=acc[r][:, :], in0=t[:, :], in1=GB[r][:, br, :], op=ALU.mult)
            else:
                c.call("dve", "tensor_tensor", [t, (GB[r], br)], [t], out=t[:, :], in0=t[:, :], in1=GB[r][:, br, :], op=ALU.mult)
                if getattr(self, "dbg", None) is not None:
                    c.ld("sp", self.dbg[br, self._hd * 64:(self._hd + 1) * 64, self._qc], t[:, :], [t], [(self.dbg, (br, self._hd, self._qc.start))])
                c.call("dve", "tensor_tensor", [t, acc[r]], [acc[r]], out=acc[r][:, :], in0=t[:, :], in1=acc[r][:, :], op=ALU.add)

        for gq in range(4):
            c.ld("sp", KS[64:128, :], kvT_d[512 + gq * 64: 512 + (gq + 1) * 64, :], [kvT_d], [(KS, "k")])
            c.ld("sp", KW[64:128, :], kvT_d[1024 + gq * 64: 1024 + (gq + 1) * 64, :], [kvT_d], [KW])
            for cc in range(0, NKC, 8):
                c.ld("sp", VS[:, cc:cc + 8, :], vtok_d[cc * 128:(cc + 8) * 128, gq * 64:(gq + 1) * 64].rearrange("(c p) d -> p c d", p=128),
                     [vtok_d], [(VS, cc)])
                c.ld("sp", VW[:, cc:cc + 8, :], vtok_d[cc * 128:(cc + 8) * 128, 256 + gq * 64: 256 + (gq + 1) * 64].rearrange("(c p) d -> p c d", p=128),
                     [vtok_d], [(VW, cc)])
            for r in range(4):
                hd = gq * 4 + r
                c.ld("sp", QS[r][64:128, :], qT_d[hd * 64:(hd + 1) * 64, :], [qT_d], [(QS[r], "q")])
            for qt in range(NQ):
                qc = slice(qt * T, (qt + 1) * T)
                for r in range(4):
                    hd = gq * 4 + r
                    for br in range(3):
                        row = hd * 3 + br
                        c.ld("sp", GB[r][:, br, :], gT_d[row:row + 1, qc].to_broadcast([64, T]), [gT_d], [(GB[r], br)])
                for r in range(4):
                    pden, pov = pdens[bsel[0] % 2], povs[bsel[0] % 2]
                    bsel[0] += 1
                    for ncx in range(NC2):
                        p = nst()
                        c.mm(p[:, :], kcmpT[64:128, gq, ncx * 128:(ncx + 1) * 128], QS[r][64:128, qc], True, True,
                             [(kcmpT, gq), (QS[r], "q")], [p])
                        e = ex[cnt[0] % 3]
                        c.act(e[:, :], p[:, :], AF.Exp, [p], [e])
                        c.call("dve", "tensor_tensor", [e, vis], [(Pc, (r, ncx))], out=Pc[:, r, ncx, :], in0=e[:, :], in1=vis[:, ncx, qc], op=ALU.mult)
                    for ncx in range(NC2):
                        c.mm(pden[:, :], self.onesb[:, :], Pc[:, r, ncx, :], ncx == 0, ncx == NC2 - 1, [self.onesb, (Pc, (r, ncx))], [pden])
                    for ncx in range(NC2):
                        c.mm(pov[0:64, :], vcmp[:, gq, ncx, :], Pc[:, r, ncx, :], ncx == 0, ncx == NC2 - 1, [(vcmp, gq), (Pc, (r, ncx))], [pov])
                    cnt[0] += 1
                    rd = rden[cnt[0] % 2]
                    c.act(rd[:, :], pden[:, :], AF.Ln, [pden, tiny], [rd], bias=tiny[:, 0:1], scale=1.0)
                    c.act(rd[:, :], rd[:, :], AF.Exp, [rd], [rd], scale=-1.0)
                    t = tmp[cnt[0] % 2]
                    c.call("dve", "tensor_tensor", [pov, rd], [t], out=t[:, :], in0=pov[0:64, :], in1=rd[0:64, :], op=ALU.mult)
                    c.call("dve", "tensor_tensor", [t, (GB[r], 0)], [acc[r]], out=acc[r][:, :], in0=t[:, :], in1=GB[r][:, 0, :], op=ALU.mult)
                    if getattr(self, "dbg", None) is not None:
                        c.ld("sp", self.dbg[0, (gq * 4 + r) * 64:(gq * 4 + r + 1) * 64, qc], acc[r][:, :], [acc[r]], [(self.dbg, (0, gq * 4 + r, qt))])
                    for ncx in range(NC2):
                        c.call("pool", "tensor_tensor", [(Pc, (r, ncx)), rd], [(Pn, (r, ncx))], out=Pn[:, r, ncx, :], in0=Pc[:, r, ncx, :],
                               in1=rd[:, :], op=ALU.mult)
                for qs in range(4):
                    qq = slice(qs * 128, (qs + 1) * 128)
                    i = 0
                    for r in range(4):
                        for ncx in range(NC2):
                            c.mm(pim[:, 0:64], Pn[:, r, ncx, qq], ov[:, ncx, :], i == 0, i == 4 * NC2 - 1, [(Pn, (r, ncx)), ov], [pim])
                            i += 1
                    ti = qt * 4 + qs
                    c.call("dve", "tensor_tensor", [pim, Fc], [imp2], out=imp2[:, :], in0=pim[:, 0:64], in1=Fc[:, ti, :], op=ALU.max)
                    c.call("dve", "tensor_tensor", [imp2, Uc], [imp2], out=imp2[:, :], in0=imp2[:, :], in1=Uc[:, ti, :], op=ALU.min)
                    c.call("dve", "max", [imp2], [m8], out=m8[:, 0:8], in_=imp2[:, :])
                    c.call("dve", "match_replace", [imp2, m8], [impt], out=impt[:, :], in_to_replace=m8[:, 0:8], in_values=imp2[:, :], imm_value=-2.0)
                    c.call("dve", "max", [impt], [m8], out=m8[:, 8:16], in_=impt[:, :])
                    c.call("dve", "tensor_scalar", [m8], [thr], out=thr[:, :], in0=m8[:, 15:16], scalar1=-0.5, scalar2=None, op0=ALU.max)
                    c.call("dve", "tensor_scalar", [imp2, thr], [negm], out=negm[:, :], in0=imp2[:, :], scalar1=thr[:, 0:1], scalar2=1.0,
                           op0=ALU.is_ge, op1=ALU.subtract)
                    c.call("dve", "tensor_scalar", [negm], [negm], out=negm[:, :], in0=negm[:, :], scalar1=30000.0, scalar2=None, op0=ALU.mult)
                    c.op("pe", (lambda o_=ptp[0:64, 128:256], i_=negm[:, :], id_=identf[:, :]: nc.tensor.transpose(o_, i_, id_)),
                         reads=[negm, identf], writes=[ptp])
                    for r in range(4):
                        dst = QS[r][0:64, qt * T + qs * 128: qt * T + (qs + 1) * 128]
                        if r % 2 == 0:
                            c.act(dst, ptp[0:64, 128:256], AF.Copy, [ptp], [(QS[r], ("m", qt, qs))])
                        else:
                            c.call("dve", "tensor_copy", [ptp], [(QS[r], ("m", qt, qs))], out=dst, in_=ptp[0:64, 128:256])
                LA = 2
                for r in range(4):
                    hd = gq * 4 + r
                    self._hd, self._qc = hd, qc
                    for br in (1, 2):
                        if br == 1:
                            kcs = list(range(4 * (qt + 1)))
                        else:
                            kcs = [kc for kc in range(4 * qt - 4, 4 * qt + 4) if kc >= 0]
                        n = len(kcs)
                        pden, pov = pdens[bsel[0] % 2], povs[bsel[0] % 2]
                        Ps = {}

                        def front(i, br=br, kcs=kcs, r=r):
                            kc = kcs[i]
                            p = nst()
                            slot = cnt[0] % 3
                            if br == 1:
                                c.mm(p[:, :], KS[:, kc * 128:(kc + 1) * 128], QS[r][:, qc], True, True,
                                     [KS, (QS[r], "q")] + [(QS[r], ("m", qt, s_)) for s_ in range(4)], [p])
                                if kc >= 4 * qt:
                                    e = ex[slot]
                                    c.act(e[:, :], p[:, :], AF.Exp, [p], [e])
                                    P = Pt[slot]
                                    c.call("pool", "tensor_tensor", [e, wm], [P], out=P[:, :], in0=e[:, :], in1=wm[:, 4 + kc - 4 * qt, :], op=ALU.mult)
                                else:
                                    P = Pt[slot]
                                    c.act(P[:, :], p[:, :], AF.Exp, [p], [P])
                            else:
                                c.mm(p[:, :], KW[64:128, kc * 128:(kc + 1) * 128], QS[r][64:128, qc], True, True, [KW, (QS[r], "q")], [p])
                                e = ex[slot]
                                c.act(e[:, :], p[:, :], AF.Exp, [p], [e])
                                P = Pt[slot]
                                c.call("dve", "tensor_tensor", [e, wm], [P], out=P[:, :], in0=e[:, :], in1=wm[:, kc - (4 * qt - 4), :], op=ALU.mult)
                            Ps[i] = P

                        for i in range(min(LA, n)):
                            front(i)
                        for i in range(n):
                            if i + LA < n:
                                front(i + LA)
                            P = Ps[i]
                            V_ = VS if br == 1 else VW
                            c.mm(pden[0:64, :], self.onesb[:, 0:64], P[:, :], i == 0, i == n - 1, [self.onesb, P], [pden])
                            c.mm(pov[0:64, :], V_[:, kcs[i], :], P[:, :], i == 0, i == n - 1, [V_, P], [pov])
                        finish(r, br, False)
                    o_ = oh[r % 2]
                    c.act(o_[:, :], acc[r][:, :], AF.Copy, [acc[r]], [o_])
                    c.ld("sp", oT_d[hd * 64:(hd + 1) * 64, qc], o_[:, :], [o_], [(oT_d, (hd, qt))])
    with c.phase():
        wo = c.sbuf("s_wo", [128, KC, D], BF16)
        self.load_w(wo, lambda kc, cs, cn: wo[:, kc, cs:cs + cn], dr["nsa_w_o"], 0, KC, 0, D)
        ot = [c.sbuf(f"s_ot{i}", [128, KC, T], BF16) for i in range(2)]
        xt = [c.sbuf(f"s_xx{i}", [128, KC, T], F32) for i in range(2)]
        ps = [c.psum(f"s_os{i}", [128, 512]) for i in range(4)]
        def o_load(tg):
            o, x = ot[tg % 2], xt[tg % 2]
            for k in range(KC):
                c.ld("sp", o[:, k, :], oT_d[k * 128:(k + 1) * 128, tg * T:(tg + 1) * T], [oT_d], [(o, k)])
                c.ld("sp", x[:, k, :], xs[k * 128:(k + 1) * 128, tg * T:(tg + 1) * T], [(xs, (k, tg))], [(x, k)])
        o_load(0)
        for tg in range(NQ):
            o, x = ot[tg % 2], xt[tg % 2]
            cs = slice(tg * T, (tg + 1) * T)
            if tg + 1 < NQ:
                o_load(tg + 1)
            for dc in range(KC):
                p = ps[dc % 4]
                for k in range(KC):
                    c.mm(p[:, :], wo[:, k, dc * 128:(dc + 1) * 128], o[:, k, :], k == 0, k == KC - 1, [wo, (o, k)], [p])
                c.call("dve", "tensor_tensor", [p, (x, dc)], [(x, dc)], out=x[:, dc, :], in0=p[:, :], in1=x[:, dc, :], op=ALU.add)
                c.ld("sp", xs[dc * 128:(dc + 1) * 128, cs], x[:, dc, :], [(x, dc)], [(xs, (dc, tg))])


M.nsa = _nsa


S_FULL = 4096
_WNAMES = ["ffn1_norm", "ffn1_w_in", "ffn1_w_out", "mix_norm", "xattn_norm", "mem_norm", "xattn_w_q", "xattn_w_kv", "xattn_w_o",
           "ffn2_norm", "ffn2_w_in", "ffn2_w_out", "pool_w", "pool_b", "pool_scale", "nsa_w_in", "nsa_cmp_pos", "nsa_cmp_w1",
           "nsa_cmp_w2", "nsa_w_o", "gla_w_in", "gla_w_gate_up", "gla_b_gate", "gla_norm", "gla_w_o", "conv_w_in", "conv_b_in",
           "conv_dw", "conv_b_dw", "conv_ln_g", "conv_ln_b", "conv_w_out", "conv_b_out", "final_norm"]


def _host_consts(S):
    import ml_dtypes
    inv = np.zeros((128, 4, 512), np.float32)
    for gi, wn in enumerate((2, 4, 8, 16)):
        inv[:, gi, :] = 1.0 / np.minimum(np.arange(512) + 1, wn)
    sm = np.ones((128, 512), np.float32)
    sm[:, ::64] = 0
    ii = np.arange(128)
    gm = ((ii[:, None] // 64 == ii[None, :] // 64) & (ii[:, None] <= ii[None, :])).astype(np.float32)
    ident = np.eye(128, dtype=np.float32).astype(ml_dtypes.bfloat16)
    NKC, NCP = S // 128, S // 16
    NC2, NCMP, NSL = NCP // 128, S // 16 - 1, S // 64
    bf = ml_dtypes.bfloat16
    key = np.arange(S)
    E = np.zeros((64, S), np.float32)
    E[key // 64, key] = 1.0
    n = np.arange(NCP).reshape(NC2, 128)
    q = np.arange(S)
    vis = ((16 * n[:, :, None] + 31) <= q[None, None, :]) & (n[:, :, None] < NCMP)
    vis = vis.transpose(1, 0, 2).astype(np.float32)
    p = np.arange(128)[:, None, None]
    dd = np.arange(8)[None, :, None]
    f = np.arange(512)[None, None, :]
    rel = 128 * (dd - 4) + p
    wm = ((rel <= f) & (rel > f - 512)).astype(np.float32)
    t = np.arange(S)
    m = np.arange(64)
    cur = t // 64
    forced = (m[None, :] == 0) | (m[None, :] == cur[:, None]) | (m[None, :] == cur[:, None] - 1)
    valid = (m[None, :] * 64 <= t[:, None]) & (m[None, :] < NSL)
    Fm = np.where(forced, 1e4, 0.0).astype(np.float32)
    Um = np.where(valid, 1e30, -1.0).astype(np.float32)
    Fm = Fm.reshape(NKC, 128, 64).transpose(1, 0, 2)
    Um = Um.reshape(NKC, 128, 64).transpose(1, 0, 2)
    cs_ = np.arange(NCP) * 16
    ss_ = np.arange(64) * 64
    ov = np.clip(np.minimum(cs_[:, None] + 32, ss_[None, :] + 64) - np.maximum(cs_[:, None], ss_[None, :]), 0, None) / 32.0
    ov[NCMP:, :] = 0
    ov[:, NSL:] = 0
    ov = ov.reshape(NC2, 128, 64).transpose(1, 0, 2)
    return dict(pool_inv=inv, scanmask=sm, gmask=gm, ident=ident, nsa_E=E.astype(bf), nsa_vis=np.ascontiguousarray(vis).astype(bf),
                nsa_wm=wm.astype(bf), nsa_F=np.ascontiguousarray(Fm), nsa_U=np.ascontiguousarray(Um),
                nsa_ov=np.ascontiguousarray(ov).astype(bf), identf=np.eye(128, dtype=np.float32))


def build_program(S=S_FULL, with_nsa=True):
    nc = bass.Bass("TRN2", target_bir_lowering=False)
    c = Ctx(nc)
    shapes = dict(
        ffn1_norm=[4, D], ffn1_w_in=[4, D, 2 * FF], ffn1_w_out=[4, FF, D], mix_norm=[4, D], xattn_norm=[4, D], mem_norm=[4, D],
        xattn_w_q=[4, D, D], xattn_w_kv=[4, D, 2 * D], xattn_w_o=[4, D, D], ffn2_norm=[4, D], ffn2_w_in=[4, D, 2 * FF],
        ffn2_w_out=[4, FF, D], pool_w=[1024, 256], pool_b=[D], pool_scale=[D], nsa_w_in=[D, 2608], nsa_cmp_pos=[2, 32, 64],
        nsa_cmp_w1=[2, 2048, 256], nsa_cmp_w2=[2, 256, 64], nsa_w_o=[D, D], gla_w_in=[D, 3088], gla_w_gate_up=[16, 512],
        gla_b_gate=[512], gla_norm=[256], gla_w_o=[D, D], conv_w_in=[D, 2 * D], conv_b_in=[2 * D], conv_dw_l=[128, KC, 31],
        conv_b_dw=[D], conv_ln_g=[D], conv_ln_b=[D], conv_w_out=[D, D], conv_b_out=[D], final_norm=[D])
    dr = {k: c.dram(k, v, F32, kind="ExternalInput") for k, v in shapes.items()}
    xin = c.dram("xT", [D, S], F32, kind="ExternalInput")
    memT = c.dram("memT", [D, 256], F32, kind="ExternalInput")
    inv_d = c.dram("pool_inv", [128, 4, 512], F32, kind="ExternalInput")
    cst = dict(scanmask=c.dram("scanmask", [128, 512], F32, kind="ExternalInput"),
               gmask=c.dram("gmask", [128, 128], F32, kind="ExternalInput"),
               ident=c.dram("ident", [128, 128], BF16, kind="ExternalInput"),
               nsa_E=c.dram("nsa_E", [64, S], BF16, kind="ExternalInput"),
               nsa_vis=c.dram("nsa_vis", [128, S // 2048, S], BF16, kind="ExternalInput"),
               nsa_wm=c.dram("nsa_wm", [128, 8, 512], BF16, kind="ExternalInput"),
               nsa_F=c.dram("nsa_F", [128, S // 128, 64], F32, kind="ExternalInput"),
               nsa_U=c.dram("nsa_U", [128, S // 128, 64], F32, kind="ExternalInput"),
               nsa_ov=c.dram("nsa_ov", [128, S // 2048, 64], BF16, kind="ExternalInput"),
               identf=c.dram("identf", [128, 128], F32, kind="ExternalInput"))
    xs = c.dram("xs", [D, S], F32)
    uT = c.dram("uT", [D, 32 + S], F32)
    out = c.dram("outT", [D, S], F32, kind="ExternalOutput")
    m = M(c, S)
    m.consts()

    class Sub:
        pass

    def lw(name, l):
        T = dr[name]
        v = Tile(c, f"{name}{l}", T.t[l], "dram")
        return v

    cur = xin
    for l in range(4):
        m.ffn(cur, xs, dr["ffn1_norm"].t[l], lw("ffn1_w_in", l), lw("ffn1_w_out", l), l)
        cur = xs
        g_ap = dr["mix_norm"].t[l]
        if l == 0:
            m.pool(xs, g_ap, dr["pool_w"], dr["pool_b"].t, dr["pool_scale"].t, inv_d)
        elif l == 1:
            if with_nsa:
                m.nsa(xs, g_ap, dr, cst)
        elif l == 2:
            m.gla(xs, g_ap, dr["gla_w_in"], dr["gla_w_gate_up"], dr["gla_b_gate"].t, dr["gla_norm"].t, dr["gla_w_o"], cst)
        else:
            m.conv(xs, g_ap, dr["conv_w_in"], dr["conv_b_in"].t, dr["conv_dw_l"], dr["conv_b_dw"].t, dr["conv_ln_g"].t,
                   dr["conv_ln_b"].t, dr["conv_w_out"], dr["conv_b_out"].t, uT)
        m.xattn(xs, memT, dr["xattn_norm"].t[l], dr["mem_norm"].t[l], lw("xattn_w_q", l), lw("xattn_w_kv", l), lw("xattn_w_o", l))
        m.ffn(xs, xs, dr["ffn2_norm"].t[l], lw("ffn2_w_in", l), lw("ffn2_w_out", l), l)
    m.final_norm(xs, out, dr["final_norm"].t)
    c.emit()
    return nc, c


def kernel(**inputs):
    S = S_FULL
    x = np.asarray(inputs["x"], np.float32)
    mem = np.asarray(inputs["mem"], np.float32)
    B = x.shape[0]
    nc, c = build_program(S)
    shared = {}
    for k in _WNAMES:
        a = np.ascontiguousarray(np.asarray(inputs[k], np.float32))
        if k in ("pool_w",):
            a = a.reshape(1024, 256)
        elif k in ("pool_b", "pool_scale", "nsa_w_in", "nsa_cmp_pos", "nsa_cmp_w1", "nsa_cmp_w2", "nsa_w_o", "gla_w_in",
                   "gla_w_gate_up", "gla_b_gate", "gla_norm", "gla_w_o", "conv_w_in", "conv_b_in", "conv_b_dw", "conv_ln_g",
                   "conv_ln_b", "conv_w_out", "conv_b_out"):
            a = a[0]
        if k == "conv_dw":
            shared["conv_dw_l"] = np.ascontiguousarray(a[0].T.reshape(KC, 128, 31).transpose(1, 0, 2))
            continue
        shared[k] = np.ascontiguousarray(a)
    shared.update(_host_consts(S))
    in_maps = []
    for b in range(B):
        mp = dict(shared)
        mp["xT"] = np.ascontiguousarray(x[b].T)
        mp["memT"] = np.ascontiguousarray(mem[b].T)
        in_maps.append(mp)
    res = run_bass_kernel_spmd(nc, in_maps, core_ids=list(range(B)))
    outp = np.stack([np.ascontiguousarray(res.results[b]["outT"].T) for b in range(B)], 0)
    return outp.astype(np.float32)
```
